# Optimizing a Trainium2 kernel written in Bass

```python
import math
import jax, jax.numpy as jnp
from jax import lax
import numpy as np

D_MODEL = 4096
BATCH = 32
SEQ = 256
DEPTH = 2
DEC_BATCH = 4
DEC_SEQ = 1024
PAST_LEN = 512

GRID_W = 64
SSD_INNER = D_MODEL
SSD_HEAD_DIM = 64
SSD_HEADS = SSD_INNER // SSD_HEAD_DIM
SSD_GROUPS = 8
SSD_STATE = 128
SSD_CONV = 3
SSD_CHUNK = 128
SSD_XBC = SSD_INNER + 2 * SSD_GROUPS * SSD_STATE
SC_WIDTH = D_MODEL // 2
SC_CONV = 3
CF_WIDTH = D_MODEL // 2
CF_CONV = 31
N_BRANCH = 3
OFF_Z = 0
OFF_XBC = OFF_Z + SSD_INNER
OFF_DT = OFF_XBC + SSD_XBC
OFF_SC = OFF_DT + 2 * SSD_HEADS
OFF_CF = OFF_SC + 3 * SC_WIDTH
OFF_GATE = OFF_CF + 2 * CF_WIDTH
IN_COLS = OFF_GATE + N_BRANCH * D_MODEL
N_EXPERTS = 16
N_EXPERT_GROUPS = 4
EXPERTS_PER_GROUP = N_EXPERTS // N_EXPERT_GROUPS
TOP_K = 2
D_EXPERT = D_MODEL // 4
EPS = 1e-6

kernel_name = 'hybrid_ssd_conv_moe_diffusion_step'


def rmsnorm(x, g):
    xf = x.astype(jnp.float32)
    y = xf * lax.rsqrt(jnp.mean(xf * xf, axis=-1, keepdims=True) + EPS)
    return (y * g.astype(jnp.float32)).astype(x.dtype)


def layernorm(x, g, b):
    xf = x.astype(jnp.float32)
    mu = jnp.mean(xf, axis=-1, keepdims=True)
    xc = xf - mu
    var = jnp.mean(xc * xc, axis=-1, keepdims=True)
    y = xc * lax.rsqrt(var + EPS) * g.astype(jnp.float32) + b.astype(jnp.float32)
    return y.astype(x.dtype)


def dwconv_seq(u, w, bias):
    k, ch = w.shape
    y = lax.conv_general_dilated(u, w.reshape(k, 1, ch).astype(u.dtype), (1,), [(k // 2, k // 2)],
                                 dimension_numbers=('NWC', 'WIO', 'NWC'), feature_group_count=ch)
    return y + bias.astype(u.dtype)


def dwconv_grid(u, w, bias, rows, vertical):
    n, seq_len, ch = u.shape
    k = w.shape[0]
    ug = u.reshape(n, rows, GRID_W, ch)
    if vertical:
        kern = w.reshape(k, 1, 1, ch)
        pad = [(k // 2, k // 2), (0, 0)]
    else:
        kern = w.reshape(1, k, 1, ch)
        pad = [(0, 0), (k // 2, k // 2)]
    y = lax.conv_general_dilated(ug, kern.astype(u.dtype), (1, 1), pad,
                                 dimension_numbers=('NHWC', 'HWIO', 'NHWC'), feature_group_count=ch)
    return y.reshape(n, seq_len, ch) + bias.astype(u.dtype)


def ssd_chunked(x, dt, a, bmat, cmat, h0):
    b, seq_len, nh, hp = x.shape
    g, n = bmat.shape[2], bmat.shape[3]
    hg = nh // g
    q = SSD_CHUNK
    nc = seq_len // q
    f32 = jnp.float32
    xc = x.astype(f32).reshape(b, nc, q, g, hg, hp)
    dtc = dt.astype(f32).reshape(b, nc, q, g, hg)
    bc = bmat.astype(f32).reshape(b, nc, q, g, n)
    cc = cmat.astype(f32).reshape(b, nc, q, g, n)
    a_cum = jnp.cumsum(dtc * a.astype(f32).reshape(g, hg), axis=2)
    seg = a_cum[:, :, :, None] - a_cum[:, :, None, :]
    mask = jnp.tril(jnp.ones((q, q), dtype=bool))[:, :, None, None]
    decay = jnp.exp(jnp.where(mask, seg, -jnp.inf))
    cb = jnp.einsum('bctgn,bcsgn->bctsg', cc, bc)
    wts = cb[..., None] * decay * dtc[:, :, None]
    y_intra = jnp.einsum('bctsgh,bcsghp->bctghp', wts, xc)
    decay_end = jnp.exp(a_cum[:, :, -1:] - a_cum)
    states = jnp.einsum('bcsgh,bcsgn,bcsghp->bcghpn', decay_end * dtc, bc, xc)
    chunk_decay = jnp.exp(a_cum[:, :, -1])

    def step(h, inp):
        st, dec = inp
        return dec[..., None, None] * h + st, h

    h_init = h0.astype(f32).reshape(b, g, hg, hp, n)
    h_final, h_enter = lax.scan(step, h_init, (jnp.moveaxis(states, 1, 0), jnp.moveaxis(chunk_decay, 1, 0)))
    h_enter = jnp.moveaxis(h_enter, 0, 1)
    y_inter = jnp.einsum('bctgn,bcghpn->bctghp', cc, h_enter) * jnp.exp(a_cum)[..., None]
    y = (y_intra + y_inter).reshape(b, seq_len, nh, hp)
    return y, h_final.reshape(b, nh, hp, n)


def hybrid_mixer(h, p, l, h0, rows):
    b, seq_len, _ = h.shape
    f32 = jnp.float32
    proj = jnp.einsum('bld,dk->blk', h, p['w_in'][l])
    z = proj[..., OFF_Z:OFF_XBC]
    xbc = proj[..., OFF_XBC:OFF_DT]
    dt_raw = proj[..., OFF_DT:OFF_SC].reshape(b, seq_len, 2, SSD_HEADS)
    sc = proj[..., OFF_SC:OFF_CF]
    cf = proj[..., OFF_CF:OFF_GATE]
    gates = jax.nn.sigmoid(proj[..., OFF_GATE:].astype(f32)).astype(h.dtype).reshape(b, seq_len, N_BRANCH, D_MODEL)

    xbc = jax.nn.silu(dwconv_seq(xbc, p['ssd_conv_w'][l], p['ssd_conv_b'][l]))
    gn = SSD_GROUPS * SSD_STATE
    xs = xbc[..., :SSD_INNER].reshape(b, seq_len, SSD_HEADS, SSD_HEAD_DIM)
    bm = xbc[..., SSD_INNER:SSD_INNER + gn].reshape(b, seq_len, SSD_GROUPS, SSD_STATE)
    cm = xbc[..., SSD_INNER + gn:].reshape(b, seq_len, SSD_GROUPS, SSD_STATE)
    dt = jax.nn.softplus(dt_raw.astype(f32) + p['ssd_dt_bias'][l].astype(f32))
    a = -jnp.exp(p['ssd_a_log'][l].astype(f32))
    rev = lambda t: jnp.flip(t, axis=1)
    y_f, h_f = ssd_chunked(xs, dt[:, :, 0], a[0], bm, cm, h0[:, 0])
    y_b, h_b = ssd_chunked(rev(xs), rev(dt[:, :, 1]), a[1], rev(bm), rev(cm), h0[:, 1])
    y = y_f + rev(y_b) + p['ssd_d'][l].astype(f32)[:, None] * xs.astype(f32)
    y = y.reshape(b, seq_len, SSD_INNER) * jax.nn.silu(z.astype(f32))
    y = rmsnorm(y, p['ssd_norm_g'][l]).astype(h.dtype)
    o_ssd = jnp.einsum('blk,kd->bld', y, p['ssd_out'][l])

    bg, cg, hv = jnp.split(sc, 3, axis=-1)
    u = cg * hv
    if rows is None:
        u = dwconv_seq(u, p['sc_conv_w'][l], p['sc_conv_b'][l])
    else:
        u = dwconv_grid(u, p['sc_conv_w'][l], p['sc_conv_b'][l], rows, vertical=False)
    o_sc = jnp.einsum('blk,kd->bld', bg * u, p['sc_out'][l])

    ga, gb = jnp.split(cf, 2, axis=-1)
    u = ga * jax.nn.sigmoid(gb)
    if rows is None:
        u = dwconv_seq(u, p['cf_conv_w'][l], p['cf_conv_b'][l])
    else:
        u = dwconv_grid(u, p['cf_conv_w'][l], p['cf_conv_b'][l], rows, vertical=True)
    u = jax.nn.silu(layernorm(u, p['cf_ln_g'][l], p['cf_ln_b'][l]))
    o_cf = jnp.einsum('blk,kd->bld', u, p['cf_out'][l])

    merged = gates[:, :, 0] * o_ssd + gates[:, :, 1] * o_sc + gates[:, :, 2] * o_cf
    out = jnp.einsum('bld,de->ble', merged, p['w_o'][l])
    return out, jnp.stack([h_f, h_b], axis=1)


def grouped_moe(h, p, l):
    b, seq_len, d = h.shape
    f32 = jnp.float32
    t = h.reshape(b * seq_len, d)
    scores = jax.nn.sigmoid(jnp.einsum('td,de->te', t, p['router_w']).astype(f32))
    sel = scores + p['router_bias'].astype(f32)
    grp_score = lax.top_k(sel.reshape(-1, N_EXPERT_GROUPS, EXPERTS_PER_GROUP), TOP_K)[0].sum(-1)
    best = jnp.argmax(grp_score, axis=-1)
    in_grp = (jnp.arange(N_EXPERTS) // EXPERTS_PER_GROUP)[None, :] == best[:, None]
    _, idx = lax.top_k(jnp.where(in_grp, sel, -jnp.inf), TOP_K)
    w_sel = jnp.take_along_axis(scores, idx, axis=-1)
    w_sel = w_sel / jnp.sum(w_sel, axis=-1, keepdims=True)
    combine = jnp.sum(jax.nn.one_hot(idx, N_EXPERTS, dtype=f32) * w_sel[..., None], axis=1)
    gt = jnp.einsum('td,edf->tef', t, p['moe_w_gate'][l])
    up = jnp.einsum('td,edf->tef', t, p['moe_w_up'][l])
    act = jax.nn.silu(gt) * up * combine[..., None].astype(t.dtype)
    y = jnp.einsum('tef,efd->td', act, p['moe_w_down'][l])
    return y.reshape(b, seq_len, d)


def trunk(x, cond, h0_all, rows, p):
    states = []
    sc = jax.nn.silu(cond)
    for l in range(DEPTH):
        mod = (jnp.einsum('bd,dk->bk', sc, p['ada_w'][l]) + p['ada_b'][l])[:, None, :]
        sh1, s1, g1, sh2, s2, g2 = jnp.split(mod, 6, axis=-1)
        h = rmsnorm(x, p['norm1_g'][l]) * (1 + s1) + sh1
        out, st = hybrid_mixer(h, p, l, h0_all[:, l], rows)
        x = x + g1 * out
        h = rmsnorm(x, p['norm2_g'][l]) * (1 + s2) + sh2
        x = x + g2 * grouped_moe(h, p, l)
        states.append(st)
    return rmsnorm(x, p['final_g']), jnp.stack(states, axis=1)


def setup_inputs(seed: int = 0) -> dict:
    key = jax.random.key(seed)
    ks = jax.random.split(key, 40)
    nrm = lambda k, shape, scale: jax.random.normal(k, shape, jnp.float32) * scale
    dt0 = jnp.exp(jax.random.uniform(ks[10], (DEPTH, 2, SSD_HEADS), jnp.float32, math.log(1e-3), math.log(1e-1)))
    return {
        'x_prompt': nrm(ks[0], (BATCH, SEQ, D_MODEL), 1.0),
        'x_sample': nrm(ks[1], (DEC_BATCH, DEC_SEQ, D_MODEL), 1.0),
        'state_ssd': nrm(ks[2], (DEC_BATCH, DEPTH, 2, SSD_HEADS, SSD_HEAD_DIM, SSD_STATE), 0.5),
        'c': nrm(ks[3], (DEC_BATCH, D_MODEL), 1.0),
        'c_ctx': nrm(ks[4], (D_MODEL,), 1.0),
        'ada_w': nrm(ks[5], (DEPTH, D_MODEL, 6 * D_MODEL), 0.5 * D_MODEL ** -0.5),
        'ada_b': nrm(ks[6], (DEPTH, 6 * D_MODEL), 0.02),
        'norm1_g': 1.0 + nrm(ks[7], (DEPTH, D_MODEL), 0.02),
        'norm2_g': 1.0 + nrm(ks[8], (DEPTH, D_MODEL), 0.02),
        'w_in': nrm(ks[9], (DEPTH, D_MODEL, IN_COLS), D_MODEL ** -0.5),
        'ssd_conv_w': nrm(ks[11], (DEPTH, SSD_CONV, SSD_XBC), SSD_CONV ** -0.5),
        'ssd_conv_b': nrm(ks[12], (DEPTH, SSD_XBC), 0.02),
        'ssd_dt_bias': dt0 + jnp.log(-jnp.expm1(-dt0)),
        'ssd_a_log': jnp.log(jax.random.uniform(ks[13], (DEPTH, 2, SSD_HEADS), jnp.float32, 1.0, 16.0)),
        'ssd_d': 1.0 + nrm(ks[14], (DEPTH, SSD_HEADS), 0.1),
        'ssd_norm_g': 1.0 + nrm(ks[15], (DEPTH, SSD_INNER), 0.02),
        'ssd_out': nrm(ks[16], (DEPTH, SSD_INNER, D_MODEL), SSD_INNER ** -0.5),
        'sc_conv_w': nrm(ks[17], (DEPTH, SC_CONV, SC_WIDTH), SC_CONV ** -0.5),
        'sc_conv_b': nrm(ks[18], (DEPTH, SC_WIDTH), 0.02),
        'sc_out': nrm(ks[19], (DEPTH, SC_WIDTH, D_MODEL), SC_WIDTH ** -0.5),
        'cf_conv_w': nrm(ks[20], (DEPTH, CF_CONV, CF_WIDTH), CF_CONV ** -0.5),
        'cf_conv_b': nrm(ks[21], (DEPTH, CF_WIDTH), 0.02),
        'cf_ln_g': 1.0 + nrm(ks[22], (DEPTH, CF_WIDTH), 0.02),
        'cf_ln_b': nrm(ks[23], (DEPTH, CF_WIDTH), 0.02),
        'cf_out': nrm(ks[24], (DEPTH, CF_WIDTH, D_MODEL), CF_WIDTH ** -0.5),
        'w_o': nrm(ks[25], (DEPTH, D_MODEL, D_MODEL), D_MODEL ** -0.5),
        'router_w': nrm(ks[26], (D_MODEL, N_EXPERTS), D_MODEL ** -0.5),
        'router_bias': nrm(ks[27], (N_EXPERTS,), 0.01),
        'moe_w_gate': nrm(ks[28], (DEPTH, N_EXPERTS, D_MODEL, D_EXPERT), D_MODEL ** -0.5),
        'moe_w_up': nrm(ks[29], (DEPTH, N_EXPERTS, D_MODEL, D_EXPERT), D_MODEL ** -0.5),
        'moe_w_down': nrm(ks[30], (DEPTH, N_EXPERTS, D_EXPERT, D_MODEL), D_EXPERT ** -0.5),
        'final_g': 1.0 + nrm(ks[31], (D_MODEL,), 0.02),
    }


def reference(x_prompt, x_sample, state_ssd, c, c_ctx, ada_w, ada_b, norm1_g, norm2_g, w_in,
              ssd_conv_w, ssd_conv_b, ssd_dt_bias, ssd_a_log, ssd_d, ssd_norm_g, ssd_out,
              sc_conv_w, sc_conv_b, sc_out, cf_conv_w, cf_conv_b, cf_ln_g, cf_ln_b, cf_out, w_o,
              router_w, router_bias, moe_w_gate, moe_w_up, moe_w_down, final_g):
    p = dict(ada_w=ada_w, ada_b=ada_b, norm1_g=norm1_g, norm2_g=norm2_g, w_in=w_in,
             ssd_conv_w=ssd_conv_w, ssd_conv_b=ssd_conv_b, ssd_dt_bias=ssd_dt_bias, ssd_a_log=ssd_a_log,
             ssd_d=ssd_d, ssd_norm_g=ssd_norm_g, ssd_out=ssd_out, sc_conv_w=sc_conv_w, sc_conv_b=sc_conv_b,
             sc_out=sc_out, cf_conv_w=cf_conv_w, cf_conv_b=cf_conv_b, cf_ln_g=cf_ln_g, cf_ln_b=cf_ln_b,
             cf_out=cf_out, w_o=w_o, router_w=router_w, router_bias=router_bias, moe_w_gate=moe_w_gate,
             moe_w_up=moe_w_up, moe_w_down=moe_w_down, final_g=final_g)
    h0_ctx = jnp.zeros((x_prompt.shape[0], DEPTH, 2, SSD_HEADS, SSD_HEAD_DIM, SSD_STATE), jnp.float32)
    y_prompt, new_state_ssd = trunk(x_prompt, c_ctx[None, :], h0_ctx, None, p)
    grid_rows = x_sample.shape[1] // GRID_W
    y_sample, _ = trunk(x_sample, c, state_ssd, grid_rows, p)
    return (y_prompt, y_sample, new_state_ssd)
```

```python
import numpy as np
from contextlib import ExitStack
from collections import defaultdict
import concourse.bass as bass
import concourse.mybir as mybir
from concourse.bass_utils import run_bass_kernel_spmd

F32 = mybir.dt.float32
BF16 = mybir.dt.bfloat16
AF = mybir.ActivationFunctionType
ALU = mybir.AluOpType
AX = mybir.AxisListType

D = 4096
KC = 32
DEPTH = 2
GRID_W = 64
OFF_Z = 0
OFF_XBC = 4096
OFF_DT = OFF_XBC + 6144
OFF_SC = OFF_DT + 128
OFF_CF = OFF_SC + 6144
OFF_GATE = OFF_CF + 4096
IN_COLS = OFF_GATE + 3 * D
NE = 16
DE = 1024
EPS = 1e-6
PSEQ = 256


class Stage:
    NS = 6

    _uid = [0]
    _sets = [None, None]
    _es = None

    def __init__(self, nc, name):
        self.nc = nc
        Stage._uid[0] += 1
        name = f"{name}{Stage._uid[0]}"
        self.name = name
        self.es = ExitStack()
        self.ops = {e: [] for e in ("pe", "act", "dve", "pool", "sp")}
        if Stage._sets[0] is None:
            cs = {e: Stage._es.enter_context(nc.semaphore(f"g_c{e}")) for e in ("pe", "act", "dve", "pool")}
            ds = {q: [Stage._es.enter_context(nc.semaphore(f"g_d{q}{i}")) for i in range(self.NS)]
                  for q in ("sp", "pool")}
            Stage._sets[0] = (cs, ds)
            Stage._cnt = ({e: 0 for e in cs}, {q: [0] * self.NS for q in ds})
        self.csem, self.dsem = Stage._sets[0]
        self.ccnt, self.dcnt = Stage._cnt
        self.drr = {q: 0 for q in self.dsem}
        self.lastw = {}
        self.readers = defaultdict(list)
        self.nsb = 0

    def sb(self, shape, dt, name=None):
        self.nsb += 1
        return self.es.enter_context(self.nc.sbuf_tensor(f"{self.name}_{name or 't'}{self.nsb}", list(shape), dt))

    def _deps(self, reads, writes):
        toks = []
        for k in reads:
            t = self.lastw.get(k)
            if t is not None:
                toks.append(t)
        for k in writes:
            t = self.lastw.get(k)
            if t is not None:
                toks.append(t)
            toks.extend(self.readers.get(k, ()))
        return toks

    def _commit(self, tok, reads, writes):
        for k in writes:
            self.lastw[k] = tok
            self.readers[k] = []
        for k in reads:
            self.readers[k].append(tok)

    def op(self, eng, fn, reads=(), writes=()):
        toks = self._deps(reads, writes)
        self.ccnt[eng] += 1
        tok = ("c", eng, self.ccnt[eng])
        self.ops[eng].append((toks, [fn], (self.csem[eng], 1)))
        self._commit(tok, reads, writes)

    def pe(self, fns, reads=(), writes=()):
        toks = self._deps(reads, writes)
        self.ccnt["pe"] += 1
        tok = ("c", "pe", self.ccnt["pe"])
        self.ops["pe"].append((toks, list(fns), (self.csem["pe"], 1)))
        self._commit(tok, reads, writes)

    def dma(self, q, out, in_, reads=(), writes=(), slow=False):
        toks = self._deps(reads, writes)
        s = self.drr[q] % self.NS
        self.drr[q] += 1
        prev = self.dcnt[q][s]
        if prev > 0:
            toks.append(("d", q, s, prev))
        self.dcnt[q][s] = prev + 16
        tok = ("d", q, s, prev + 16)
        if slow:
            fn = lambda e: e.dma_start(out=out, in_=in_, allow_slow_non_contiguous=True)
        else:
            fn = lambda e: e.dma_start(out=out, in_=in_)
        self.ops[q].append((toks, [fn], (self.dsem[q][s], 16)))
        self._commit(tok, reads, writes)

    def flush(self):
        nc = self.nc
        ops = self.ops

        def resolve(tok):
            if tok[0] == "c":
                return self.csem[tok[1]], tok[2], tok[1]
            return self.dsem[tok[1]][tok[2]], tok[3], None

        def emit(engname, e):
            waited = {}
            for toks, fns, (sem, inc) in ops[engname]:
                for tok in toks:
                    s, v, src = resolve(tok)
                    if engname == "pe" and src == "pe":
                        continue
                    key = id(s)
                    if waited.get(key, 0) >= v:
                        continue
                    waited[key] = v
                    e.wait_ge(s, v)
                ins = None
                for fn in fns:
                    ins = fn(e)
                ins.then_inc(sem, inc)
            if engname in self.dsem:
                for s in range(self.NS):
                    if self.dcnt[engname][s] > 0:
                        e.wait_ge(self.dsem[engname][s], self.dcnt[engname][s])

        with nc.Block() as block:
            if ops["sp"]:
                @block.sync
                def _(e):
                    emit("sp", e)
            if ops["pool"]:
                @block.gpsimd
                def _(e):
                    emit("pool", e)
            if ops["pe"]:
                @block.tensor
                def _(e):
                    emit("pe", e)
            if ops["act"]:
                @block.scalar
                def _(e):
                    emit("act", e)
            if ops["dve"]:
                @block.vector
                def _(e):
                    emit("dve", e)
        self.es.close()


def build(ROWS=16, NP=4, debug=False, depth=DEPTH, stop_after=None, only=None):
    LS = ROWS * GRID_W
    LP = NP * PSEQ
    T = LS + LP
    TBM = max(LS, LP)
    assert TBM <= 1024 and LS % 128 == 0
    nc = bass.Bass("TRN2", target_bir_lowering=False)
    Stage._uid[0] = 0
    Stage._sets = [None, None]
    Stage._es = ExitStack()

    def din(name, shape):
        return nc.dram_tensor(name, list(shape), F32, kind="ExternalInput").ap()

    skind = "ExternalOutput" if debug else "Internal"

    def scr(name, shape, dt):
        return nc.dram_tensor(name, list(shape), dt, kind=skind).ap()

    xin = din("xin", [T, D])
    cond = din("cond", [128, KC, 2])
    h0s = din("h0s", [DEPTH, 2, D, 128])
    cst = din("cst", [128, 4, 128])
    W = {}
    for nm, shp in [("ada_w", [DEPTH, D, 6 * D]), ("ada_b", [DEPTH, 6 * D]), ("norm1_g", [DEPTH, D]),
                    ("norm2_g", [DEPTH, D]), ("w_in", [DEPTH, D, IN_COLS]), ("ssd_conv_w", [DEPTH, 128, 48, 3]),
                    ("ssd_conv_b", [DEPTH, 128, 48]), ("ssd_dt_bias", [DEPTH, 128]), ("ssd_a_log", [DEPTH, 128]),
                    ("ssd_d", [DEPTH, 64]), ("ssd_norm_g", [DEPTH, D]), ("ssd_out", [DEPTH, D, D]),
                    ("sc_conv_w", [DEPTH, 128, 16, 3]), ("sc_conv_b", [DEPTH, 128, 16]), ("sc_out", [DEPTH, 2048, D]),
                    ("cf_conv_w", [DEPTH, 128, 16, 31]), ("cf_conv_b", [DEPTH, 128, 16]), ("cf_ln_g", [DEPTH, 128, 16]),
                    ("cf_ln_b", [DEPTH, 128, 16]), ("cf_out", [DEPTH, 2048, D]), ("w_o", [DEPTH, D, D]),
                    ("router_w", [D, NE]), ("router_bias", [NE]), ("moe_w_gate", [DEPTH, NE, D, DE]),
                    ("moe_w_up", [DEPTH, NE, D, DE]), ("moe_w_down", [DEPTH, NE, DE, D]), ("final_g", [D])]:
        W[nm] = din(nm, shp)
    y = nc.dram_tensor("y", [T, D], F32, kind="ExternalOutput").ap()
    nst = nc.dram_tensor("nst", [NP, DEPTH, 2, D, 128], F32, kind="ExternalOutput").ap()

    NCHM = TBM // 128
    modv = scr("modv", [DEPTH, 2, 6 * D], F32)
    hT = scr("hT", [D, TBM], BF16)
    zs = scr("zs", [TBM, D], BF16)
    xbcf = scr("xbcf", [6144, TBM], BF16)
    xbt = scr("xbt", [TBM, 5120], BF16)
    vsc = scr("vsc", [2048, TBM], BF16)
    ucf = scr("ucf", [2048, TBM], F32)
    gts = scr("gts", [3 * D, TBM], BF16)
    dts = scr("dts", [NCHM, 128, 5, 128], F32)
    acb = scr("acb", [NCHM, 2, 64, 128], F32)
    hent = scr("hent", [NCHM, 2, 128, D], BF16)
    ynf = scr("ynf", [D, TBM], BF16)
    mrg = scr("mrg", [D, TBM], BF16)
    xres = scr("xres", [T, D], F32)
    comb = scr("comb", [NE, TBM], F32)
    aT = scr("aT", [NE * DE, TBM], BF16)

    with nc.psum_tensor("PS", [128, 4096], F32) as PS:
        def bank(b, n=512):
            return PS[:, b * 512:b * 512 + n]

        def bankbf(b):
            return PS[:, b * 512:(b + 1) * 512].bitcast(BF16)

        def load_consts(st):
            c = st.sb([128, 4, 128], F32, "cst")
            st.dma("sp", c[:], cst, writes=["cst"])
            return c

        def stage_ada():
            st = Stage(nc, "ada")
            cT = st.sb([128, KC, 2], F32)
            cTb = st.sb([128, KC, 2], BF16)
            st.dma("sp", cT[:], cond, writes=["cT"])
            st.op("act", lambda e: e.activation(out=cTb[:], in_=cT[:], func=AF.Silu), reads=["cT"], writes=["cTb"])
            ws = [st.sb([128, KC, 512], BF16) for _ in range(2)]
            bs = [st.sb([2, 512], F32) for _ in range(2)]
            os_ = [st.sb([2, 512], F32) for _ in range(2)]
            it = 0
            for l in range(depth):
                wv = W["ada_w"][l].rearrange("(k p) c -> p k c", p=128)
                for cb in range(48):
                    s = it % 2
                    cs = slice(cb * 512, (cb + 1) * 512)
                    st.dma("pool", ws[s][:], wv[:, :, cs], writes=[("w", s)])
                    st.dma("sp", bs[s][:], W["ada_b"][l, cs].partition_broadcast(2), writes=[("b", s)])
                    pb = bank(it % 8)
                    st.pe([(lambda e, k=k, s=s, pb=pb: e.matmul(pb[0:2, :], lhsT=cTb[:, k, :], rhs=ws[s][:, k, :],
                                                                  start=(k == 0), stop=(k == KC - 1)))
                           for k in range(KC)], reads=["cTb", ("w", s)], writes=[("ps", it % 8)])
                    st.op("dve", lambda e, s=s, pb=pb: e.tensor_tensor(out=os_[s][:], in0=pb[0:2, :], in1=bs[s][:],
                                                                         op=ALU.add),
                          reads=[("ps", it % 8), ("b", s)], writes=[("o", s)])
                    st.dma("sp", modv[l, :, cs], os_[s][:], reads=[("o", s)])
                    it += 1
            st.flush()

        def stage_norm(l, which, grp):
            tok0, TB, seqs, ci = grp
            st = Stage(nc, f"n{which}")
            c = load_consts(st)
            epsb = st.sb([128, 1], F32)
            st.op("dve", lambda e: e.memset(epsb[:], EPS), writes=["epsb"])
            identb = st.sb([128, 128], BF16)
            st.op("dve", lambda e: e.tensor_copy(out=identb[:], in_=c[:, 0, :]), reads=["cst"], writes=["idb"])
            G = st.sb([128, D], F32)
            if which == "f":
                st.dma("sp", G[:], W["final_g"].partition_broadcast(128), writes=["G"])
                xsrc = xres
            else:
                S = st.sb([128, D], F32)
                SH = st.sb([128, D], F32)
                gname = "norm1_g" if which == 1 else "norm2_g"
                o = 0 if which == 1 else 3 * D
                st.dma("sp", G[:], W[gname][l].partition_broadcast(128), writes=["G"])
                st.dma("sp", S[:], modv[l, ci, o + D:o + 2 * D].partition_broadcast(128), writes=["S"])
                st.dma("sp", SH[:], modv[l, ci, o:o + D].partition_broadcast(128), writes=["SH"])
                st.op("dve", lambda e: e.scalar_tensor_tensor(out=G[:], in0=S[:], scalar=1.0, in1=G[:],
                                                              op0=ALU.add, op1=ALU.mult), reads=["S", "G"], writes=["G"])
                xsrc = xin if (l == 0 and which == 1) else xres
            xs = [st.sb([128, D], F32) for _ in range(2)]
            junk = st.sb([128, D], BF16)
            ss = [st.sb([128, 1], F32) for _ in range(2)]
            hb = [st.sb([128, D], BF16) for _ in range(2)]
            hTs = [st.sb([128, KC, 128], BF16) for _ in range(2)]
            for i in range(TB // 128):
                s = i % 2
                rows = slice(tok0 + i * 128, tok0 + (i + 1) * 128)
                st.dma("sp", xs[s][:], xsrc[rows, :], reads=[("x", i)], writes=[("xs", s)])
                st.op("dve", lambda e, s=s: e.memset(ss[s][:], 0.0), writes=[("ss", s)])
                st.op("act", lambda e, s=s: e.activation(out=junk[:], in_=xs[s][:], func=AF.Square,
                                                         accum_out=ss[s][:, 0:1]),
                      reads=[("xs", s)], writes=["junk", ("ss", s)])
                st.op("act", lambda e, s=s: e.activation(out=ss[s][:], in_=ss[s][:], func=AF.Sqrt, scale=1.0 / D, bias=epsb[:, 0:1]),
                      reads=[("ss", s), "epsb"], writes=[("ss", s)])
                st.op("dve", lambda e, s=s: e.reciprocal(out=ss[s][:], in_=ss[s][:]), reads=[("ss", s)], writes=[("ss", s)])
                st.op("dve", lambda e, s=s: e.scalar_tensor_tensor(out=xs[s][:], in0=xs[s][:], scalar=ss[s][:, 0:1],
                                                                   in1=G[:], op0=ALU.mult, op1=ALU.mult),
                      reads=[("xs", s), ("ss", s), "G"], writes=[("xs", s)])
                if which == "f":
                    st.dma("sp", y[rows, :], xs[s][:], reads=[("xs", s)])
                    continue
                st.op("dve", lambda e, s=s: e.tensor_tensor(out=hb[s][:], in0=xs[s][:], in1=SH[:], op=ALU.add),
                      reads=[("xs", s), "SH"], writes=[("hb", s)])
                for q in range(4):
                    b = (i * 4 + q) % 8
                    pb = bankbf(b)
                    st.pe([(lambda e, r=r, q=q, s=s, pb=pb: e.transpose(out=pb[:, r * 128:(r + 1) * 128],
                                                                          in_=hb[s][:, (q * 8 + r) * 128:(q * 8 + r + 1) * 128],
                                                                          identity=identb[:]))
                           for r in range(8)], reads=[("hb", s), "idb"], writes=[("ps", b)])
                    eng = "act" if q % 2 == 0 else "dve"
                    dst = hTs[s][:, q * 8:(q + 1) * 8, :].rearrange("p a b -> p (a b)")
                    if eng == "act":
                        st.op("act", lambda e, dst=dst, pb=pb: e.activation(out=dst, in_=pb, func=AF.Copy),
                              reads=[("ps", b)], writes=[("hTs", s, q)])
                    else:
                        st.op("dve", lambda e, dst=dst, pb=pb: e.tensor_copy(out=dst, in_=pb),
                              reads=[("ps", b)], writes=[("hTs", s, q)])
                st.dma("sp", hT.rearrange("(k p) t -> p k t", p=128)[:, :, i * 128:(i + 1) * 128], hTs[s][:],
                       reads=[("hTs", s, q) for q in range(4)], writes=[("hTd", i)])
            st.flush()

        def load_resident(st, src, kc, TB, key):
            r = st.sb([128, kc, TB], BF16, "res")
            st.dma("sp", r[:], src.rearrange("(k p) t -> p k t", p=128)[:, :, 0:TB], writes=[key])
            return r

        def ntile(TB):
            NT = min(512, TB)
            return NT, TB // NT

        def stage_proj(l, grp):
            tok0, TB, seqs, ci = grp
            is_s = (ci == 0)
            st = Stage(nc, "pj")
            c = load_consts(st)
            hTr = load_resident(st, hT, KC, TB, "hTr")
            NT, ntt = ntile(TB)
            win = W["w_in"][l].rearrange("(k p) c -> p k c", p=128)
            ws = [st.sb([128, KC, 512], BF16, "w") for _ in range(2)]
            wit = [0]
            pit = [0]

            def wblock(colranges):
                s = wit[0] % 2
                wit[0] += 1
                o = 0
                for (c0, n) in colranges:
                    st.dma("pool", ws[s][:, :, o:o + n], win[:, :, c0:c0 + n], writes=[("w", s, o // 128 + i) for i in range(n // 128)])
                    o += n
                return s

            def fm_mm(s, wo):
                b0 = (pit[0] * ntt) % 8
                pit[0] += 1
                keys = [("ps", b0 + n) for n in range(ntt)]
                fns = []
                for n in range(ntt):
                    for k in range(KC):
                        fns.append(lambda e, n=n, k=k, s=s, wo=wo, b0=b0: e.matmul(
                            bank(b0 + n, NT), lhsT=ws[s][:, k, wo * 128:(wo + 1) * 128],
                            rhs=hTr[:, k, n * NT:(n + 1) * NT], start=(k == 0), stop=(k == KC - 1)))
                st.pe(fns, reads=["hTr", ("w", s, wo)], writes=keys)
                return PS[:, b0 * 512:b0 * 512 + TB] if ntt > 1 or NT == 512 else PS[:, b0 * 512:b0 * 512 + TB], keys

            wdt = st.sb([128, KC, 128], BF16)
            st.dma("pool", wdt[:], win[:, :, OFF_DT:OFF_DT + 128], writes=["wdt"])
            dtb = st.sb([128, 128], F32)
            abc = st.sb([128, 128], F32)
            st.dma("sp", dtb[:], W["ssd_dt_bias"][l].partition_broadcast(128), writes=["dtb"])
            st.dma("sp", abc[:], W["ssd_a_log"][l].partition_broadcast(128), writes=["abc"])
            st.op("act", lambda e: e.activation(out=abc[:], in_=abc[:], func=AF.Exp), reads=["abc"], writes=["abc"])
            st.op("dve", lambda e: e.tensor_scalar(out=abc[:], in0=abc[:], scalar1=-1.0, scalar2=None, op0=ALU.mult),
                  reads=["abc"], writes=["abc"])
            Ds = [st.sb([128, 5, 128], F32) for _ in range(2)]
            tmp = [[st.sb([128, 128], F32) for _ in range(6)] for _ in range(2)]
            for cch in range(TB // 128):
                s = cch % 2
                B0 = s * 4
                P0, P1, P2, P3 = bank(B0, 128), bank(B0 + 1, 128), bank(B0 + 2, 128), bank(B0 + 3, 128)
                k0, k1, k2, k3 = [("ps", B0 + i) for i in range(4)]
                x1, ax, e1, l1, dtA, t2 = tmp[s]
                Dt = Ds[s]
                tk = lambda n: ("tmp", s, n)
                st.pe([(lambda e, k=k, cch=cch, P0=P0: e.matmul(P0, lhsT=hTr[:, k, cch * 128:(cch + 1) * 128],
                                                               rhs=wdt[:, k, :], start=(k == 0), stop=(k == KC - 1)))
                       for k in range(KC)], reads=["hTr", "wdt"], writes=[k0])
                st.op("dve", lambda e, x1=x1, P0=P0: e.tensor_tensor(out=x1[:], in0=P0, in1=dtb[:], op=ALU.add),
                      reads=[k0, "dtb"], writes=[tk(0)])
                st.op("act", lambda e, x1=x1, ax=ax: e.activation(out=ax[:], in_=x1[:], func=AF.Abs),
                      reads=[tk(0)], writes=[tk(1)])
                st.op("act", lambda e, ax=ax, e1=e1: e.activation(out=e1[:], in_=ax[:], func=AF.Exp, scale=-1.0),
                      reads=[tk(1)], writes=[tk(2)])
                st.op("act", lambda e, l1=l1, e1=e1: e.activation(out=l1[:], in_=e1[:], func=AF.Ln, bias=1.0),
                      reads=[tk(2)], writes=[tk(3)])
                st.op("dve", lambda e, Dt=Dt, x1=x1, l1=l1: e.scalar_tensor_tensor(out=Dt[:, 0, :], in0=x1[:], scalar=0.0,
                                                                                  in1=l1[:], op0=ALU.max, op1=ALU.add),
                      reads=[tk(0), tk(3)], writes=[("D", s, 0)])
                st.op("dve", lambda e, Dt=Dt, dtA=dtA: e.tensor_tensor(out=dtA[:], in0=Dt[:, 0, :], in1=abc[:], op=ALU.mult),
                      reads=[("D", s, 0), "abc"], writes=[tk(4)])
                st.pe([lambda e, P1=P1, dtA=dtA: e.matmul(P1[:, 0:64], lhsT=c[:, 1, :], rhs=dtA[:, 0:64], start=True, stop=True),
                       lambda e, P1=P1, dtA=dtA: e.matmul(P1[:, 64:128], lhsT=c[:, 2, :], rhs=dtA[:, 64:128], start=True, stop=True)],
                      reads=["cst", tk(4)], writes=[k1])
                st.pe([lambda e, P2=P2, dtA=dtA: e.matmul(P2[0:64, :], lhsT=dtA[:, 0:64], rhs=c[:, 1, :], start=True, stop=True),
                       lambda e, P2=P2, dtA=dtA: e.matmul(P2[64:128, :], lhsT=dtA[:, 64:128], rhs=c[:, 2, :], start=True, stop=True)],
                      reads=["cst", tk(4)], writes=[k2])
                st.pe([lambda e, P3=P3, dtA=dtA: e.matmul(P3, lhsT=c[:, 3, :], rhs=dtA[:], start=True, stop=True)],
                      reads=["cst", tk(4)], writes=[k3])
                st.op("act", lambda e, Dt=Dt, P1=P1: e.activation(out=Dt[:, 1, :], in_=P1, func=AF.Copy, scale=-1.0),
                      reads=[k1], writes=[("D", s, 1)])
                st.op("act", lambda e, Dt=Dt, P1=P1: e.activation(out=Dt[:, 2, :], in_=P1, func=AF.Exp),
                      reads=[k1], writes=[("D", s, 2)])
                st.op("dve", lambda e, Dt=Dt, P3=P3, t2=t2: e.tensor_tensor(out=t2[:], in0=P3, in1=Dt[:, 1, :], op=ALU.add),
                      reads=[k3, ("D", s, 1)], writes=[tk(5)])
                st.op("act", lambda e, t2=t2: e.activation(out=t2[:], in_=t2[:], func=AF.Exp), reads=[tk(5)], writes=[tk(5)])
                st.op("dve", lambda e, Dt=Dt, t2=t2: e.tensor_tensor(out=Dt[:, 3, :], in0=t2[:], in1=Dt[:, 0, :], op=ALU.mult),
                      reads=[tk(5), ("D", s, 0)], writes=[("D", s, 3)])
                st.op("act", lambda e, Dt=Dt, P3=P3: e.activation(out=Dt[:, 4, :], in_=P3, func=AF.Exp),
                      reads=[k3], writes=[("D", s, 4)])
                st.op("act", lambda e, ax=ax, P2=P2: e.activation(out=ax[:], in_=P2, func=AF.Copy), reads=[k2], writes=[tk(1)])
                st.dma("sp", dts[cch], Dt[:], reads=[("D", s, i) for i in range(5)])
                st.dma("sp", acb[cch].rearrange("d h t -> (d h) t"), ax[:], reads=[tk(1)])

            zo = [st.sb([128, 512], BF16) for _ in range(2)]
            zi = 0
            for cb in range(8):
                s = wblock([(OFF_Z + cb * 512, 512)])
                for tt in range(TB // 128):
                    b = pit[0] % 8
                    pit[0] += 1
                    st.pe([(lambda e, k=k, tt=tt, s=s, b=b: e.matmul(bank(b), lhsT=hTr[:, k, tt * 128:(tt + 1) * 128],
                                                                    rhs=ws[s][:, k, :], start=(k == 0), stop=(k == KC - 1)))
                           for k in range(KC)], reads=["hTr"] + [("w", s, i) for i in range(4)], writes=[("ps", b)])
                    z = zi % 2
                    zi += 1
                    st.op("act", lambda e, z=z, b=b: e.activation(out=zo[z][:], in_=bank(b), func=AF.Silu),
                          reads=[("ps", b)], writes=[("zo", z)])
                    st.dma("sp", zs[tt * 128:(tt + 1) * 128, cb * 512:(cb + 1) * 512], zo[z][:], reads=[("zo", z)])
            pit[0] = 0

            cw = st.sb([128, 48, 3], F32)
            cbi = st.sb([128, 48], F32)
            st.dma("sp", cw[:], W["ssd_conv_w"][l], writes=["cw"])
            st.dma("sp", cbi[:], W["ssd_conv_b"][l], writes=["cbi"])
            t0s = [st.sb([128, TB], F32) for _ in range(2)]
            accs = [st.sb([128, TB], F32) for _ in range(2)]
            obs = [st.sb([128, TB], BF16) for _ in range(2)]
            nseg = len(seqs)
            L = seqs[0][1]
            ei = [0]

            def conv3(t0, acc, w3, bias, j, seglen, rk, wk):
                v = lambda a: a[:].rearrange("p (s l) -> p s l", l=seglen)
                st.op("dve", lambda e: e.tensor_scalar(out=acc[:], in0=t0[:], scalar1=w3[:, j, 1:2], scalar2=bias[:, j:j + 1],
                                                       op0=ALU.mult, op1=ALU.add), reads=rk, writes=wk)
                st.op("dve", lambda e: e.scalar_tensor_tensor(out=v(acc)[:, :, 1:], in0=v(t0)[:, :, 0:seglen - 1],
                                                              scalar=w3[:, j, 0:1], in1=v(acc)[:, :, 1:],
                                                              op0=ALU.mult, op1=ALU.add), reads=rk + wk, writes=wk)
                st.op("dve", lambda e: e.scalar_tensor_tensor(out=v(acc)[:, :, 0:seglen - 1], in0=v(t0)[:, :, 1:],
                                                              scalar=w3[:, j, 2:3], in1=v(acc)[:, :, 0:seglen - 1],
                                                              op0=ALU.mult, op1=ALU.add), reads=rk + wk, writes=wk)

            for blk in range(12):
                s = wblock([(OFF_XBC + blk * 512, 512)])
                for wo in range(4):
                    j = blk * 4 + wo
                    ps, keys = fm_mm(s, wo)
                    q = ei[0] % 2
                    ei[0] += 1
                    st.op("act", lambda e, q=q, ps=ps: e.activation(out=t0s[q][:], in_=ps, func=AF.Copy),
                          reads=keys, writes=[("t0", q)])
                    conv3(t0s[q], accs[q], cw, cbi, j, L, ["cw", "cbi", ("t0", q)], [("acc", q)])
                    st.op("act", lambda e, q=q: e.activation(out=obs[q][:], in_=accs[q][:], func=AF.Silu),
                          reads=[("acc", q)], writes=[("ob", q)])
                    st.dma("sp", xbcf[j * 128:(j + 1) * 128, 0:TB], obs[q][:], reads=[("ob", q)])

            scw = st.sb([128, 16, 3], F32)
            scb = st.sb([128, 16], F32)
            st.dma("sp", scw[:], W["sc_conv_w"][l], writes=["scw"])
            st.dma("sp", scb[:], W["sc_conv_b"][l], writes=["scb"])
            bgs = [st.sb([128, TB], F32) for _ in range(2)]
            cgs = [st.sb([128, TB], F32) for _ in range(2)]
            sseg = GRID_W if is_s else PSEQ
            for j in range(16):
                s = wblock([(OFF_SC + j * 128, 128), (OFF_SC + 2048 + j * 128, 128), (OFF_SC + 4096 + j * 128, 128)])
                q = j % 2
                ps, keys = fm_mm(s, 0)
                st.op("act", lambda e, q=q, ps=ps: e.activation(out=bgs[q][:], in_=ps, func=AF.Copy), reads=keys, writes=[("bg", q)])
                ps, keys = fm_mm(s, 1)
                st.op("act", lambda e, q=q, ps=ps: e.activation(out=cgs[q][:], in_=ps, func=AF.Copy), reads=keys, writes=[("cg", q)])
                ps, keys = fm_mm(s, 2)
                st.op("dve", lambda e, q=q, ps=ps: e.tensor_tensor(out=t0s[q][:], in0=ps, in1=cgs[q][:], op=ALU.mult),
                      reads=keys + [("cg", q)], writes=[("t0", q)])
                conv3(t0s[q], accs[q], scw, scb, j, sseg, ["scw", "scb", ("t0", q)], [("acc", q)])
                st.op("dve", lambda e, q=q: e.tensor_tensor(out=obs[q][:], in0=accs[q][:], in1=bgs[q][:], op=ALU.mult),
                      reads=[("acc", q), ("bg", q)], writes=[("ob", q)])
                st.dma("sp", vsc[j * 128:(j + 1) * 128, 0:TB], obs[q][:], reads=[("ob", q)])

            cfw = st.sb([128, 16, 31], F32)
            cfb = st.sb([128, 16], F32)
            st.dma("sp", cfw[:], W["cf_conv_w"][l], writes=["cfw"])
            st.dma("sp", cfb[:], W["cf_conv_b"][l], writes=["cfb"])
            for j in range(16):
                s = wblock([(OFF_CF + j * 128, 128), (OFF_CF + 2048 + j * 128, 128)])
                q = j % 2
                ps, keys = fm_mm(s, 0)
                st.op("act", lambda e, q=q, ps=ps: e.activation(out=bgs[q][:], in_=ps, func=AF.Copy), reads=keys, writes=[("bg", q)])
                ps, keys = fm_mm(s, 1)
                st.op("act", lambda e, q=q, ps=ps: e.activation(out=cgs[q][:], in_=ps, func=AF.Sigmoid), reads=keys, writes=[("cg", q)])
                u = t0s[q]
                acc = accs[q]
                st.op("dve", lambda e, q=q, u=u: e.tensor_tensor(out=u[:], in0=bgs[q][:], in1=cgs[q][:], op=ALU.mult),
                      reads=[("bg", q), ("cg", q)], writes=[("t0", q)])
                rk = ["cfw", "cfb", ("t0", q)]
                wk = [("acc", q)]
                st.op("dve", lambda e, u=u, acc=acc, j=j: e.tensor_scalar(out=acc[:], in0=u[:], scalar1=cfw[:, j, 15:16],
                                                                        scalar2=cfb[:, j:j + 1], op0=ALU.mult, op1=ALU.add),
                      reads=rk, writes=wk)
                for d in range(-15, 16):
                    if d == 0:
                        continue
                    k = d + 15
                    if is_s:
                        if abs(d) >= ROWS:
                            continue
                        sh = GRID_W * abs(d)
                        if d > 0:
                            oa, ia = acc[:, 0:TB - sh], u[:, sh:TB]
                        else:
                            oa, ia = acc[:, sh:TB], u[:, 0:TB - sh]
                    else:
                        v = lambda a: a[:].rearrange("p (s l) -> p s l", l=PSEQ)
                        ad = abs(d)
                        if d > 0:
                            oa, ia = v(acc)[:, :, 0:PSEQ - ad], v(u)[:, :, ad:PSEQ]
                        else:
                            oa, ia = v(acc)[:, :, ad:PSEQ], v(u)[:, :, 0:PSEQ - ad]
                    st.op("dve", lambda e, oa=oa, ia=ia, j=j, k=k: e.scalar_tensor_tensor(
                        out=oa, in0=ia, scalar=cfw[:, j, k:k + 1], in1=oa, op0=ALU.mult, op1=ALU.add),
                          reads=rk + wk, writes=wk)
                st.dma("sp", ucf[j * 128:(j + 1) * 128, 0:TB], acc[:], reads=wk)

            for blk in range(24):
                s = wblock([(OFF_GATE + blk * 512, 512)])
                for wo in range(4):
                    j = blk * 4 + wo
                    ps, keys = fm_mm(s, wo)
                    q = ei[0] % 2
                    ei[0] += 1
                    st.op("act", lambda e, q=q, ps=ps: e.activation(out=obs[q][:], in_=ps, func=AF.Sigmoid),
                          reads=keys, writes=[("ob", q)])
                    st.dma("sp", gts[j * 128:(j + 1) * 128, 0:TB], obs[q][:], reads=[("ob", q)])
            st.flush()

        def stage_tr(grp):
            tok0, TB, seqs, ci = grp
            st = Stage(nc, "tr")
            c = load_consts(st)
            identb = st.sb([128, 128], BF16)
            st.op("dve", lambda e: e.tensor_copy(out=identb[:], in_=c[:, 0, :]), reads=["cst"], writes=["idb"])
            nti = TB // 128
            xs = [st.sb([128, TB], BF16) for _ in range(3)]
            ts = [st.sb([128, 8, 128], BF16) for _ in range(3)]
            xv = xbt.rearrange("(i p) c -> p i c", p=128)
            for j in range(40):
                s = j % 3
                b = j % 8
                st.dma("sp", xs[s][:], xbcf[j * 128:(j + 1) * 128, 0:TB], writes=[("xs", s)])
                pb = bankbf(b)
                st.pe([(lambda e, i=i, s=s, pb=pb: e.transpose(out=pb[:, i * 128:(i + 1) * 128],
                                                              in_=xs[s][:, i * 128:(i + 1) * 128], identity=identb[:]))
                       for i in range(nti)], reads=[("xs", s), "idb"], writes=[("ps", b)])
                dst = ts[s][:, 0:nti, :].rearrange("p a b -> p (a b)")
                if j % 2 == 0:
                    st.op("act", lambda e, dst=dst, pb=pb: e.activation(out=dst, in_=pb[:, 0:nti * 128], func=AF.Copy),
                          reads=[("ps", b)], writes=[("ts", s)])
                else:
                    st.op("dve", lambda e, dst=dst, pb=pb: e.tensor_copy(out=dst, in_=pb[:, 0:nti * 128]),
                          reads=[("ps", b)], writes=[("ts", s)])
                st.dma("sp", xv[:, 0:nti, j * 128:(j + 1) * 128], ts[s][:, 0:nti, :], reads=[("ts", s)])
            st.flush()

        def stage_ssd1(l, grp):
            tok0, TB, seqs, ci = grp
            is_s = (ci == 0)
            st = Stage(nc, "s1")
            c = load_consts(st)
            hst = [st.sb([128, D], F32, "hst") for _ in range(2)]
            h0t = st.sb([128, KC, 128], F32)
            snb = [st.sb([128, D], BF16) for _ in range(2)]
            xbs = [st.sb([128, 5120], BF16) for _ in range(2)]
            Dts = [st.sb([128, 5, 128], F32) for _ in range(2)]
            xsc = [st.sb([128, D], BF16) for _ in range(2)]
            it = 0
            pbi = 0
            for si, (s0, L) in enumerate(seqs):
                nch = L // 128
                c0 = s0 // 128
                for dr in range(2):
                    H = hst[dr]
                    hk = ("hst", dr)
                    if is_s:
                        st.dma("sp", h0t[:], h0s[l, dr].rearrange("(q p) n -> p q n", p=128), reads=[], writes=["h0t"])
                        for q4 in range(8):
                            b = pbi % 8
                            pbi += 1
                            st.pe([(lambda e, r=r, q4=q4, b=b: e.transpose(out=bank(b)[:, r * 128:(r + 1) * 128],
                                                                           in_=h0t[:, q4 * 4 + r, :], identity=c[:, 0, :]))
                                   for r in range(4)], reads=["h0t", "cst"], writes=[("ps", b)])
                            st.op("act", lambda e, H=H, q4=q4, b=b: e.activation(out=H[:, q4 * 512:(q4 + 1) * 512], in_=bank(b), func=AF.Copy),
                                  reads=[("ps", b)], writes=[hk])
                    else:
                        st.op("dve", lambda e, H=H: e.memset(H[:], 0.0), writes=[hk])
                    order = range(nch) if dr == 0 else range(nch - 1, -1, -1)
                    for cc in order:
                        cch = c0 + cc
                        s = it % 2
                        it += 1
                        st.op("act", lambda e, H=H, s=s: e.activation(out=snb[s][:], in_=H[:], func=AF.Copy),
                              reads=[hk], writes=[("snb", s)])
                        st.dma("sp", hent[cch, dr], snb[s][:], reads=[("snb", s)])
                        st.dma("sp", xbs[s][:], xbt[cch * 128:(cch + 1) * 128, :], writes=[("xb", s)])
                        st.dma("sp", Dts[s][:], dts[cch], writes=[("Dt", s)])
                        st.op("dve", lambda e, s=s, dr=dr: e.tensor_tensor(
                            out=xsc[s][:].rearrange("p (h q) -> p h q", q=64),
                            in0=xbs[s][:, 0:D].rearrange("p (h q) -> p h q", q=64),
                            in1=Dts[s][:, 3, dr * 64:(dr + 1) * 64].unsqueeze(2).broadcast_to([128, 64, 64]), op=ALU.mult),
                              reads=[("xb", s), ("Dt", s)], writes=[("xsc", s)])
                        for g in range(8):
                            b = pbi % 8
                            pbi += 1
                            st.pe([lambda e, s=s, g=g, b=b: e.matmul(bank(b), lhsT=xbs[s][:, D + g * 128:D + (g + 1) * 128],
                                                                  rhs=xsc[s][:, g * 512:(g + 1) * 512], start=True, stop=True)],
                                  reads=[("xb", s), ("xsc", s)], writes=[("ps", b)])
                            Hg = H[:, g * 512:(g + 1) * 512]
                            st.op("dve", lambda e, Hg=Hg, s=s, dr=dr, g=g: e.tensor_tensor(
                                out=Hg.rearrange("p (h q) -> p h q", q=64), in0=Hg.rearrange("p (h q) -> p h q", q=64),
                                in1=Dts[s][:, 4, dr * 64 + g * 8:dr * 64 + (g + 1) * 8].unsqueeze(2).broadcast_to([128, 8, 64]),
                                op=ALU.mult), reads=[hk, ("Dt", s)], writes=[hk])
                            st.op("dve", lambda e, Hg=Hg, b=b: e.tensor_tensor(out=Hg, in0=Hg, in1=bank(b), op=ALU.add),
                                  reads=[hk, ("ps", b)], writes=[hk])
                    if not is_s:
                        fo = h0t
                        for q4 in range(8):
                            b = pbi % 8
                            pbi += 1
                            st.pe([(lambda e, r=r, q4=q4, b=b, H=H: e.transpose(out=bank(b)[:, r * 128:(r + 1) * 128],
                                                                                in_=H[:, (q4 * 4 + r) * 128:(q4 * 4 + r + 1) * 128],
                                                                                identity=c[:, 0, :]))
                                   for r in range(4)], reads=[hk, "cst"], writes=[("ps", b)])
                            st.op("act", lambda e, q4=q4, b=b: e.activation(
                                out=fo[:, q4 * 4:(q4 + 1) * 4, :].rearrange("p a b -> p (a b)"), in_=bank(b), func=AF.Copy),
                                  reads=[("ps", b)], writes=["h0t"])
                        st.dma("sp", nst[si, l, dr].rearrange("(q p) n -> p q n", p=128), fo[:], reads=["h0t"])
            st.flush()

        def stage_ssd2(l, grp):
            tok0, TB, seqs, ci = grp
            st = Stage(nc, "s2")
            c = load_consts(st)
            epsb = st.sb([128, 1], F32)
            st.op("dve", lambda e: e.memset(epsb[:], EPS), writes=["epsb"])
            identb = st.sb([128, 128], BF16)
            st.op("dve", lambda e: e.tensor_copy(out=identb[:], in_=c[:, 0, :]), reads=["cst"], writes=["idb"])
            d64 = st.sb([128, 64], F32)
            Dbc = st.sb([128, D], F32)
            gnb = st.sb([128, D], F32)
            st.dma("sp", d64[:], W["ssd_d"][l].partition_broadcast(128), writes=["d64"])
            st.dma("sp", gnb[:], W["ssd_norm_g"][l].partition_broadcast(128), writes=["gnb"])
            st.op("dve", lambda e: e.tensor_copy(out=Dbc[:].rearrange("p (h q) -> p h q", q=64),
                                                 in_=d64[:].unsqueeze(2).broadcast_to([128, 64, 64])),
                  reads=["d64"], writes=["Dbc"])
            xb = st.sb([128, 5120], BF16)
            Dt = st.sb([128, 5, 128], F32)
            zsb = st.sb([128, D], BF16)
            Bf = st.sb([128, 8, 128], BF16)
            Cf = st.sb([128, 8, 128], BF16)
            he = [st.sb([128, D], BF16) for _ in range(2)]
            ysb = st.sb([128, D], F32)
            ynb = st.sb([128, D], BF16)
            ynT = st.sb([128, KC, 128], BF16)
            junk = st.sb([128, D], BF16)
            ss = st.sb([128, 1], F32)
            cbm = [[st.sb([128, 128], F32) for _ in range(2)] for _ in range(2)]
            acbc = [[st.sb([128, 8, 128], F32) for _ in range(2)] for _ in range(2)]
            NR = 32
            segs = [st.sb([128, 128], F32) for _ in range(NR)]
            Wts = [st.sb([128, 128], BF16) for _ in range(NR)]
            t1s = [st.sb([128, 512], F32) for _ in range(2)]
            t2s = [st.sb([128, 512], F32) for _ in range(2)]
            Bv = xbcf[4096:5120].rearrange("(g p) t -> p g t", p=128)
            Cv = xbcf[5120:6144].rearrange("(g p) t -> p g t", p=128)
            ri = 0
            gi = 0
            for cch in range(TB // 128):
                tsl = slice(cch * 128, (cch + 1) * 128)
                st.dma("sp", xb[:], xbt[tsl, :], writes=["xb"])
                st.dma("sp", Dt[:], dts[cch], writes=["Dt"])
                st.dma("sp", zsb[:], zs[tsl, :], writes=["zsb"])
                st.dma("sp", Bf[:], Bv[:, :, tsl], writes=["Bf"])
                st.dma("sp", Cf[:], Cv[:, :, tsl], writes=["Cf"])
                st.dma("sp", he[0][:], hent[cch, 0], writes=[("he", 0)])
                st.dma("sp", he[1][:], hent[cch, 1], writes=[("he", 1)])
                for g in range(8):
                    p = gi % 2
                    gi += 1
                    bCB, bY, bIf, bIb = p, 2 + p, 4 + p, 6 + p
                    CBp = bank(bCB, 128)
                    st.pe([lambda e, g=g, CBp=CBp: e.matmul(CBp, lhsT=Bf[:, g, :], rhs=Cf[:, g, :], start=True, stop=True)],
                          reads=["Bf", "Cf"], writes=[("ps", bCB)])
                    for dr in range(2):
                        st.op("dve", lambda e, p=p, dr=dr, CBp=CBp: e.tensor_tensor(out=cbm[p][dr][:], in0=CBp, in1=c[:, 1 + dr, :],
                                                                                    op=ALU.mult),
                              reads=[("ps", bCB), "cst"], writes=[("cbm", p, dr)])
                        st.dma("sp", acbc[p][dr][:].rearrange("p a b -> p (a b)"),
                               acb[cch, dr, g * 8:(g + 1) * 8, :].rearrange("a b -> (a b)").partition_broadcast(128),
                               writes=[("acbc", p, dr)])
                    items = [(hh, dr) for hh in range(8) for dr in range(2)]
                    rs = []
                    for (hh, dr) in items:
                        r = ri % NR
                        ri += 1
                        rs.append(r)
                        col = dr * 64 + g * 8 + hh
                        st.op("dve", lambda e, r=r, p=p, dr=dr, hh=hh, col=col: e.tensor_scalar(
                            out=segs[r][:], in0=acbc[p][dr][:, hh, :], scalar1=Dt[:, 1, col:col + 1], scalar2=0.0,
                            op0=ALU.add, op1=ALU.min), reads=[("acbc", p, dr), "Dt"], writes=[("seg", r)])
                    for (hh, dr), r in zip(items, rs):
                        st.op("act", lambda e, r=r: e.activation(out=segs[r][:], in_=segs[r][:], func=AF.Exp),
                              reads=[("seg", r)], writes=[("seg", r)])
                    for (hh, dr), r in zip(items, rs):
                        col = dr * 64 + g * 8 + hh
                        st.op("dve", lambda e, r=r, p=p, dr=dr, col=col: e.scalar_tensor_tensor(
                            out=Wts[r][:], in0=segs[r][:], scalar=Dt[:, 0, col:col + 1], in1=cbm[p][dr][:],
                            op0=ALU.mult, op1=ALU.mult), reads=[("seg", r), "Dt", ("cbm", p, dr)], writes=[("Wt", r)])
                    for (hh, dr), r in zip(items, rs):
                        head = g * 8 + hh
                        st.pe([lambda e, r=r, bY=bY, hh=hh, head=head, dr=dr: e.matmul(
                            bank(bY)[:, hh * 64:(hh + 1) * 64], lhsT=Wts[r][:], rhs=xb[:, head * 64:(head + 1) * 64],
                            start=(dr == 0), stop=(dr == 1))], reads=[("Wt", r), "xb"], writes=[("ps", bY)])
                    st.pe([lambda e, g=g, bIf=bIf: e.matmul(bank(bIf), lhsT=Cf[:, g, :], rhs=he[0][:, g * 512:(g + 1) * 512],
                                                         start=True, stop=True)], reads=["Cf", ("he", 0)], writes=[("ps", bIf)])
                    st.pe([lambda e, g=g, bIb=bIb: e.matmul(bank(bIb), lhsT=Cf[:, g, :], rhs=he[1][:, g * 512:(g + 1) * 512],
                                                         start=True, stop=True)], reads=["Cf", ("he", 1)], writes=[("ps", bIb)])
                    t1, t2 = t1s[p], t2s[p]
                    v3 = lambda a: a.rearrange("p (h q) -> p h q", q=64)
                    eb = lambda dr, g=g: Dt[:, 2, dr * 64 + g * 8:dr * 64 + (g + 1) * 8].unsqueeze(2).broadcast_to([128, 8, 64])
                    st.op("dve", lambda e, t1=t1, bIf=bIf, eb=eb: e.tensor_tensor(out=v3(t1[:]), in0=v3(bank(bIf)), in1=eb(0), op=ALU.mult),
                          reads=[("ps", bIf), "Dt"], writes=[("t1", p)])
                    st.op("dve", lambda e, t1=t1, bY=bY: e.tensor_tensor(out=t1[:], in0=t1[:], in1=bank(bY), op=ALU.add),
                          reads=[("ps", bY), ("t1", p)], writes=[("t1", p)])
                    st.op("dve", lambda e, t2=t2, bIb=bIb, eb=eb: e.tensor_tensor(out=v3(t2[:]), in0=v3(bank(bIb)), in1=eb(1), op=ALU.mult),
                          reads=[("ps", bIb), "Dt"], writes=[("t2", p)])
                    st.op("dve", lambda e, t1=t1, t2=t2: e.tensor_tensor(out=t1[:], in0=t1[:], in1=t2[:], op=ALU.add),
                          reads=[("t1", p), ("t2", p)], writes=[("t1", p)])
                    st.op("dve", lambda e, t2=t2, g=g: e.tensor_tensor(out=t2[:], in0=xb[:, g * 512:(g + 1) * 512],
                                                                       in1=Dbc[:, g * 512:(g + 1) * 512], op=ALU.mult),
                          reads=["xb", "Dbc", ("t2", p)], writes=[("t2", p)])
                    st.op("dve", lambda e, t1=t1, t2=t2, g=g: e.tensor_tensor(out=ysb[:, g * 512:(g + 1) * 512], in0=t1[:], in1=t2[:],
                                                                              op=ALU.add),
                          reads=[("t1", p), ("t2", p)], writes=["ysb"])
                st.op("dve", lambda e: e.tensor_tensor(out=ysb[:], in0=ysb[:], in1=zsb[:], op=ALU.mult),
                      reads=["ysb", "zsb"], writes=["ysb"])
                st.op("dve", lambda e: e.memset(ss[:], 0.0), writes=["ss"])
                st.op("act", lambda e: e.activation(out=junk[:], in_=ysb[:], func=AF.Square, accum_out=ss[:, 0:1]),
                      reads=["ysb", "ss"], writes=["junk", "ss"])
                st.op("act", lambda e: e.activation(out=ss[:], in_=ss[:], func=AF.Sqrt, scale=1.0 / D, bias=epsb[:, 0:1]),
                      reads=["ss", "epsb"], writes=["ss"])
                st.op("dve", lambda e: e.reciprocal(out=ss[:], in_=ss[:]), reads=["ss"], writes=["ss"])
                st.op("dve", lambda e: e.scalar_tensor_tensor(out=ynb[:], in0=ysb[:], scalar=ss[:, 0:1], in1=gnb[:],
                                                              op0=ALU.mult, op1=ALU.mult), reads=["ysb", "ss", "gnb"], writes=["ynb"])
                for q in range(4):
                    b = q
                    pb = bankbf(b)
                    st.pe([(lambda e, r=r, q=q, pb=pb: e.transpose(out=pb[:, r * 128:(r + 1) * 128],
                                                                  in_=ynb[:, (q * 8 + r) * 128:(q * 8 + r + 1) * 128], identity=identb[:]))
                           for r in range(8)], reads=["ynb", "idb"], writes=[("ps", b)])
                    dst = ynT[:, q * 8:(q + 1) * 8, :].rearrange("p a b -> p (a b)")
                    st.op("act", lambda e, dst=dst, pb=pb: e.activation(out=dst, in_=pb, func=AF.Copy),
                          reads=[("ps", b)], writes=[("ynT", q)])
                st.dma("sp", ynf.rearrange("(k p) t -> p k t", p=128)[:, :, tsl], ynT[:], reads=[("ynT", q) for q in range(4)])
            st.flush()

        def stage_o1(l, grp):
            tok0, TB, seqs, ci = grp
            st = Stage(nc, "o1")
            yr = load_resident(st, ynf, KC, TB, "yr")
            NT, ntt = ntile(TB)
            wv = W["ssd_out"][l].rearrange("(k p) c -> p k c", p=128)
            ws = [st.sb([128, KC, 512], BF16) for _ in range(2)]
            gt = [st.sb([128, TB], BF16) for _ in range(2)]
            ob = [st.sb([128, TB], BF16) for _ in range(2)]
            pit = 0
            for blk in range(8):
                s = blk % 2
                st.dma("pool", ws[s][:], wv[:, :, blk * 512:(blk + 1) * 512], writes=[("w", s)])
                for wo in range(4):
                    j = blk * 4 + wo
                    b0 = (pit * ntt) % 8
                    pit += 1
                    q = j % 2
                    keys = [("ps", b0 + n) for n in range(ntt)]
                    st.pe([(lambda e, n=n, k=k, s=s, wo=wo, b0=b0: e.matmul(bank(b0 + n, NT), lhsT=ws[s][:, k, wo * 128:(wo + 1) * 128],
                                                                           rhs=yr[:, k, n * NT:(n + 1) * NT], start=(k == 0), stop=(k == KC - 1)))
                           for n in range(ntt) for k in range(KC)], reads=["yr", ("w", s)], writes=keys)
                    st.dma("sp", gt[q][:], gts[j * 128:(j + 1) * 128, 0:TB], writes=[("gt", q)])
                    st.op("dve", lambda e, q=q, b0=b0: e.tensor_tensor(out=ob[q][:], in0=PS[:, b0 * 512:b0 * 512 + TB], in1=gt[q][:], op=ALU.mult),
                          reads=keys + [("gt", q)], writes=[("ob", q)])
                    st.dma("sp", mrg[j * 128:(j + 1) * 128, 0:TB], ob[q][:], reads=[("ob", q)])
            st.flush()

        def stage_o2(l, grp):
            tok0, TB, seqs, ci = grp
            st = Stage(nc, "o2")
            c = load_consts(st)
            epsb = st.sb([128, 1], F32)
            st.op("dve", lambda e: e.memset(epsb[:], EPS), writes=["epsb"])
            vr = load_resident(st, vsc, 16, TB, "vr")
            ur = st.sb([128, 16, TB], BF16)
            lg = st.sb([128, 16], F32)
            lb = st.sb([128, 16], F32)
            st.dma("sp", lg[:], W["cf_ln_g"][l], writes=["lg"])
            st.dma("sp", lb[:], W["cf_ln_b"][l], writes=["lb"])
            LT = min(256, TB)
            uin = st.sb([128, 16, LT], F32)
            usq = st.sb([128, 16, LT], F32)
            mean = st.sb([128, LT], F32)
            msq = st.sb([128, LT], F32)
            rstd = st.sb([128, LT], F32)
            tt_ = [st.sb([128, LT], F32) for _ in range(2)]
            uv = ucf.rearrange("(j p) t -> p j t", p=128)
            for ti in range(TB // LT):
                tsl = slice(ti * LT, (ti + 1) * LT)
                st.dma("sp", uin[:], uv[:, :, tsl], writes=["uin"])
                st.op("dve", lambda e: e.tensor_tensor(out=usq[:], in0=uin[:], in1=uin[:], op=ALU.mult), reads=["uin"], writes=["usq"])
                bm, be = bank(0, LT), bank(1, LT)
                st.pe([(lambda e, j=j, bm=bm: e.matmul(bm, lhsT=c[:, 3, :], rhs=uin[:, j, :], start=(j == 0), stop=(j == 15)))
                       for j in range(16)], reads=["uin", "cst"], writes=[("ps", 0)])
                st.pe([(lambda e, j=j, be=be: e.matmul(be, lhsT=c[:, 3, :], rhs=usq[:, j, :], start=(j == 0), stop=(j == 15)))
                       for j in range(16)], reads=["usq", "cst"], writes=[("ps", 1)])
                st.op("dve", lambda e, bm=bm: e.tensor_scalar(out=mean[:], in0=bm, scalar1=1.0 / 2048, scalar2=None, op0=ALU.mult),
                      reads=[("ps", 0)], writes=["mean"])
                st.op("dve", lambda e: e.tensor_tensor(out=msq[:], in0=mean[:], in1=mean[:], op=ALU.mult), reads=["mean"], writes=["msq"])
                st.op("dve", lambda e, be=be: e.scalar_tensor_tensor(out=rstd[:], in0=be, scalar=1.0 / 2048, in1=msq[:],
                                                                    op0=ALU.mult, op1=ALU.subtract), reads=[("ps", 1), "msq"], writes=["rstd"])
                st.op("act", lambda e: e.activation(out=rstd[:], in_=rstd[:], func=AF.Sqrt, bias=epsb[:, 0:1]),
                      reads=["rstd", "epsb"], writes=["rstd"])
                st.op("dve", lambda e: e.reciprocal(out=rstd[:], in_=rstd[:]), reads=["rstd"], writes=["rstd"])
                for j in range(16):
                    t = tt_[j % 2]
                    st.op("dve", lambda e, t=t, j=j: e.tensor_tensor(out=t[:], in0=uin[:, j, :], in1=mean[:], op=ALU.subtract),
                          reads=["uin", "mean"], writes=[("t", j % 2)])
                    st.op("dve", lambda e, t=t: e.tensor_tensor(out=t[:], in0=t[:], in1=rstd[:], op=ALU.mult),
                          reads=[("t", j % 2), "rstd"], writes=[("t", j % 2)])
                    st.op("act", lambda e, t=t, j=j, tsl=tsl: e.activation(out=ur[:, j, tsl], in_=t[:], func=AF.Silu,
                                                                         scale=lg[:, j:j + 1], bias=lb[:, j:j + 1]),
                          reads=[("t", j % 2), "lg", "lb"], writes=["ur"])
            NT, ntt = ntile(TB)
            sv = W["sc_out"][l].rearrange("(k p) c -> p k c", p=128)
            cv = W["cf_out"][l].rearrange("(k p) c -> p k c", p=128)
            ws = [st.sb([128, KC, 256], BF16) for _ in range(2)]
            g1 = [st.sb([128, TB], BF16) for _ in range(2)]
            g2 = [st.sb([128, TB], BF16) for _ in range(2)]
            mt = [st.sb([128, TB], BF16) for _ in range(2)]
            aa = [st.sb([128, TB], F32) for _ in range(2)]
            bb = [st.sb([128, TB], F32) for _ in range(2)]
            ob = [st.sb([128, TB], BF16) for _ in range(2)]
            pit = 0
            for blk in range(16):
                s = blk % 2
                cs = slice(blk * 256, (blk + 1) * 256)
                st.dma("pool", ws[s][:, 0:16, :], sv[:, :, cs], writes=[("wa", s)])
                st.dma("pool", ws[s][:, 16:32, :], cv[:, :, cs], writes=[("wb", s)])
                for wo in range(2):
                    j = blk * 2 + wo
                    q = j % 2
                    b0 = (pit * 2 * ntt) % 8
                    pit += 1
                    ka = [("ps", b0 + n) for n in range(ntt)]
                    kb = [("ps", b0 + ntt + n) for n in range(ntt)]
                    st.pe([(lambda e, n=n, k=k, s=s, wo=wo, b0=b0: e.matmul(bank(b0 + n, NT), lhsT=ws[s][:, k, wo * 128:(wo + 1) * 128],
                                                                           rhs=vr[:, k, n * NT:(n + 1) * NT], start=(k == 0), stop=(k == 15)))
                           for n in range(ntt) for k in range(16)], reads=["vr", ("wa", s)], writes=ka)
                    st.pe([(lambda e, n=n, k=k, s=s, wo=wo, b0=b0: e.matmul(bank(b0 + ntt + n, NT), lhsT=ws[s][:, 16 + k, wo * 128:(wo + 1) * 128],
                                                                           rhs=ur[:, k, n * NT:(n + 1) * NT], start=(k == 0), stop=(k == 15)))
                           for n in range(ntt) for k in range(16)], reads=["ur", ("wb", s)], writes=kb)
                    rows = slice(j * 128, (j + 1) * 128)
                    st.dma("sp", g1[q][:], gts[D + j * 128:D + (j + 1) * 128, 0:TB], writes=[("g1", q)])
                    st.dma("sp", g2[q][:], gts[2 * D + j * 128:2 * D + (j + 1) * 128, 0:TB], writes=[("g2", q)])
                    st.dma("sp", mt[q][:], mrg[rows, 0:TB], reads=[("mrg", j)], writes=[("mt", q)])
                    pa = PS[:, b0 * 512:b0 * 512 + TB]
                    pb = PS[:, (b0 + ntt) * 512:(b0 + ntt) * 512 + TB]
                    st.op("dve", lambda e, q=q, pa=pa: e.tensor_tensor(out=aa[q][:], in0=pa, in1=g1[q][:], op=ALU.mult),
                          reads=ka + [("g1", q)], writes=[("aa", q)])
                    st.op("dve", lambda e, q=q, pb=pb: e.tensor_tensor(out=bb[q][:], in0=pb, in1=g2[q][:], op=ALU.mult),
                          reads=kb + [("g2", q)], writes=[("bb", q)])
                    st.op("dve", lambda e, q=q: e.tensor_tensor(out=aa[q][:], in0=aa[q][:], in1=bb[q][:], op=ALU.add),
                          reads=[("aa", q), ("bb", q)], writes=[("aa", q)])
                    st.op("dve", lambda e, q=q: e.tensor_tensor(out=ob[q][:], in0=aa[q][:], in1=mt[q][:], op=ALU.add),
                          reads=[("aa", q), ("mt", q)], writes=[("ob", q)])
                    st.dma("sp", mrg[rows, 0:TB], ob[q][:], reads=[("ob", q)], writes=[("mrg", j)])
            st.flush()

        def stage_wo(l, grp):
            tok0, TB, seqs, ci = grp
            st = Stage(nc, "wo")
            mr = load_resident(st, mrg, KC, TB, "mr")
            grow = st.sb([128, D], F32)
            st.dma("sp", grow[:], modv[l, ci, 2 * D:3 * D].partition_broadcast(128), writes=["grow"])
            xsrc = xin if l == 0 else xres
            wv = W["w_o"][l].rearrange("(k p) c -> p k c", p=128)
            ws = [st.sb([128, KC, 512], BF16) for _ in range(2)]
            xt = [st.sb([128, 512], F32) for _ in range(3)]
            tt_ = [st.sb([128, 512], F32) for _ in range(3)]
            it = 0
            for cb in range(8):
                s = cb % 2
                cs = slice(cb * 512, (cb + 1) * 512)
                st.dma("pool", ws[s][:], wv[:, :, cs], writes=[("w", s)])
                for tt in range(TB // 128):
                    b = it % 8
                    q = it % 3
                    it += 1
                    rows = slice(tok0 + tt * 128, tok0 + (tt + 1) * 128)
                    st.pe([(lambda e, k=k, tt=tt, s=s, b=b: e.matmul(bank(b), lhsT=mr[:, k, tt * 128:(tt + 1) * 128], rhs=ws[s][:, k, :],
                                                                    start=(k == 0), stop=(k == KC - 1))) for k in range(KC)],
                          reads=["mr", ("w", s)], writes=[("ps", b)])
                    st.dma("sp", xt[q][:], xsrc[rows, cs], reads=[("x", tt, cb)], writes=[("xt", q)])
                    st.op("dve", lambda e, q=q, b=b, cs=cs: e.tensor_tensor(out=tt_[q][:], in0=bank(b), in1=grow[:, cs], op=ALU.mult),
                          reads=[("ps", b), "grow"], writes=[("tt", q)])
                    st.op("dve", lambda e, q=q: e.tensor_tensor(out=tt_[q][:], in0=tt_[q][:], in1=xt[q][:], op=ALU.add),
                          reads=[("tt", q), ("xt", q)], writes=[("tt", q)])
                    st.dma("sp", xres[rows, cs], tt_[q][:], reads=[("tt", q)], writes=[("x", tt, cb)])
            st.flush()

        def stage_gu(l, grp):
            tok0, TB, seqs, ci = grp
            st = Stage(nc, "gu")
            c = load_consts(st)
            hTr = load_resident(st, hT, KC, TB, "hTr")
            rw = st.sb([128, KC, NE], BF16)
            st.dma("pool", rw[:], W["router_w"].rearrange("(k p) e -> p k e", p=128), writes=["rw"])
            rb = st.sb([128, NE], F32)
            st.dma("sp", rb[:], W["router_bias"].partition_broadcast(128), writes=["rb"])
            combT = st.sb([NE, TB], F32)
            sm = [[st.sb([128, NE], F32) for _ in range(6)] for _ in range(2)]
            s4 = [[st.sb([128, 4], F32) for _ in range(10)] for _ in range(2)]
            s1 = [[st.sb([128, 1], F32) for _ in range(3)] for _ in range(2)]
            for tt in range(TB // 128):
                p = tt % 2
                b = p
                sc_, sel, ge16, m16, w_, cmb = sm[p]
                mab, nab, mcd, ncd, m1, tmn, umx, m2, gs, gmask = s4[p]
                gmax, thr, wsum = s1[p]
                K = lambda n: ("r", p, n)
                st.pe([(lambda e, k=k, tt=tt, b=b: e.matmul(bank(b)[:, 0:NE], lhsT=hTr[:, k, tt * 128:(tt + 1) * 128], rhs=rw[:, k, :],
                                                           start=(k == 0), stop=(k == KC - 1))) for k in range(KC)],
                      reads=["hTr", "rw"], writes=[("ps", b)])
                st.op("act", lambda e, sc_=sc_, b=b: e.activation(out=sc_[:], in_=bank(b)[:, 0:NE], func=AF.Sigmoid),
                      reads=[("ps", b)], writes=[K("sc")])
                st.op("dve", lambda e, sel=sel, sc_=sc_: e.tensor_tensor(out=sel[:], in0=sc_[:], in1=rb[:], op=ALU.add),
                      reads=[K("sc"), "rb"], writes=[K("sel")])
                v = lambda a, i: a[:].rearrange("p (g x) -> p g x", x=4)[:, :, i]
                def tt2(out, a, b_, op, rk, wkk):
                    st.op("dve", lambda e: e.tensor_tensor(out=out, in0=a, in1=b_, op=op), reads=rk, writes=wkk)
                tt2(mab[:], v(sel, 0), v(sel, 1), ALU.max, [K("sel")], [K("mab")])
                tt2(nab[:], v(sel, 0), v(sel, 1), ALU.min, [K("sel")], [K("nab")])
                tt2(mcd[:], v(sel, 2), v(sel, 3), ALU.max, [K("sel")], [K("mcd")])
                tt2(ncd[:], v(sel, 2), v(sel, 3), ALU.min, [K("sel")], [K("ncd")])
                tt2(m1[:], mab[:], mcd[:], ALU.max, [K("mab"), K("mcd")], [K("m1")])
                tt2(tmn[:], mab[:], mcd[:], ALU.min, [K("mab"), K("mcd")], [K("tmn")])
                tt2(umx[:], nab[:], ncd[:], ALU.max, [K("nab"), K("ncd")], [K("umx")])
                tt2(m2[:], tmn[:], umx[:], ALU.max, [K("tmn"), K("umx")], [K("m2")])
                tt2(gs[:], m1[:], m2[:], ALU.add, [K("m1"), K("m2")], [K("gs")])
                st.op("dve", lambda e, gmax=gmax, gs=gs: e.tensor_reduce(out=gmax[:], in_=gs[:], axis=AX.X, op=ALU.max),
                      reads=[K("gs")], writes=[K("gmax")])
                st.op("dve", lambda e, gmask=gmask, gs=gs, gmax=gmax: e.tensor_scalar(out=gmask[:], in0=gs[:], scalar1=gmax[:, 0:1],
                                                                                  scalar2=None, op0=ALU.is_ge),
                      reads=[K("gs"), K("gmax")], writes=[K("gmask")])
                tt2(tmn[:], gmask[:], m2[:], ALU.mult, [K("gmask"), K("m2")], [K("tmn")])
                st.op("dve", lambda e, thr=thr, tmn=tmn: e.tensor_reduce(out=thr[:], in_=tmn[:], axis=AX.X, op=ALU.add),
                      reads=[K("tmn")], writes=[K("thr")])
                st.op("dve", lambda e, ge16=ge16, sel=sel, thr=thr: e.tensor_scalar(out=ge16[:], in0=sel[:], scalar1=thr[:, 0:1],
                                                                                scalar2=None, op0=ALU.is_ge),
                      reads=[K("sel"), K("thr")], writes=[K("ge16")])
                st.op("dve", lambda e, m16=m16, ge16=ge16, gmask=gmask: e.tensor_tensor(
                    out=m16[:].rearrange("p (g x) -> p g x", x=4), in0=ge16[:].rearrange("p (g x) -> p g x", x=4),
                    in1=gmask[:].unsqueeze(2).broadcast_to([128, 4, 4]), op=ALU.mult),
                      reads=[K("ge16"), K("gmask")], writes=[K("m16")])
                tt2(w_[:], sc_[:], m16[:], ALU.mult, [K("sc"), K("m16")], [K("w")])
                st.op("dve", lambda e, wsum=wsum, w_=w_: e.tensor_reduce(out=wsum[:], in_=w_[:], axis=AX.X, op=ALU.add),
                      reads=[K("w")], writes=[K("wsum")])
                st.op("dve", lambda e, wsum=wsum: e.reciprocal(out=wsum[:], in_=wsum[:]), reads=[K("wsum")], writes=[K("wsum")])
                st.op("dve", lambda e, cmb=cmb, w_=w_, wsum=wsum: e.tensor_scalar(out=cmb[:], in0=w_[:], scalar1=wsum[:, 0:1],
                                                                              scalar2=None, op0=ALU.mult),
                      reads=[K("w"), K("wsum")], writes=[K("cmb")])
                b2 = 2 + p
                st.pe([lambda e, cmb=cmb, b2=b2: e.transpose(out=bank(b2)[0:NE, 0:128], in_=cmb[:], identity=c[:, 0, :])],
                      reads=[K("cmb"), "cst"], writes=[("ps", b2)])
                st.op("act", lambda e, tt=tt, b2=b2: e.activation(out=combT[:, tt * 128:(tt + 1) * 128], in_=bank(b2)[0:NE, 0:128],
                                                                 func=AF.Copy), reads=[("ps", b2)], writes=["combT"])
            st.dma("sp", comb[:, 0:TB], combT[:], reads=["combT"], writes=["combd"])
            NT, ntt = ntile(TB)
            ws = [st.sb([128, KC, 512], BF16) for _ in range(2)]
            cbc = [st.sb([128, TB], F32) for _ in range(2)]
            sg = [st.sb([128, TB], F32) for _ in range(2)]
            ab = [st.sb([128, TB], BF16) for _ in range(2)]
            wi = 0
            pit = 0
            for ex in range(NE):
                ce = ex % 2
                st.dma("sp", cbc[ce][:], comb[ex, 0:TB].partition_broadcast(128), reads=["combd"], writes=[("cbc", ce)])
                gv = W["moe_w_gate"][l, ex].rearrange("(k p) c -> p k c", p=128)
                uv = W["moe_w_up"][l, ex].rearrange("(k p) c -> p k c", p=128)
                for jj in range(4):
                    s = wi % 2
                    wi += 1
                    st.dma("pool", ws[s][:, :, 0:256], gv[:, :, jj * 256:(jj + 1) * 256], writes=[("wg", s)])
                    st.dma("pool", ws[s][:, :, 256:512], uv[:, :, jj * 256:(jj + 1) * 256], writes=[("wu", s)])
                    for sub in range(2):
                        j = jj * 2 + sub
                        q = j % 2
                        b0 = (pit * 2 * ntt) % 8
                        pit += 1
                        kg = [("ps", b0 + n) for n in range(ntt)]
                        ku = [("ps", b0 + ntt + n) for n in range(ntt)]
                        st.pe([(lambda e, n=n, k=k, s=s, sub=sub, b0=b0: e.matmul(bank(b0 + n, NT), lhsT=ws[s][:, k, sub * 128:(sub + 1) * 128],
                                                                                rhs=hTr[:, k, n * NT:(n + 1) * NT], start=(k == 0), stop=(k == KC - 1)))
                               for n in range(ntt) for k in range(KC)], reads=["hTr", ("wg", s)], writes=kg)
                        st.pe([(lambda e, n=n, k=k, s=s, sub=sub, b0=b0: e.matmul(bank(b0 + ntt + n, NT), lhsT=ws[s][:, k, 256 + sub * 128:256 + (sub + 1) * 128],
                                                                                rhs=hTr[:, k, n * NT:(n + 1) * NT], start=(k == 0), stop=(k == KC - 1)))
                               for n in range(ntt) for k in range(KC)], reads=["hTr", ("wu", s)], writes=ku)
                        pg = PS[:, b0 * 512:b0 * 512 + TB]
                        pu = PS[:, (b0 + ntt) * 512:(b0 + ntt) * 512 + TB]
                        st.op("act", lambda e, q=q, pg=pg: e.activation(out=sg[q][:], in_=pg, func=AF.Silu), reads=kg, writes=[("sg", q)])
                        st.op("dve", lambda e, q=q, pu=pu: e.tensor_tensor(out=sg[q][:], in0=sg[q][:], in1=pu, op=ALU.mult),
                              reads=ku + [("sg", q)], writes=[("sg", q)])
                        st.op("dve", lambda e, q=q, ce=ce: e.tensor_tensor(out=ab[q][:], in0=sg[q][:], in1=cbc[ce][:], op=ALU.mult),
                              reads=[("sg", q), ("cbc", ce)], writes=[("ab", q)])
                        st.dma("sp", aT[ex * DE + j * 128:ex * DE + (j + 1) * 128, 0:TB], ab[q][:], reads=[("ab", q)])
            st.flush()

        def stage_down(l, grp):
            tok0, TB, seqs, ci = grp
            st = Stage(nc, "dn")
            grow = st.sb([128, D], F32)
            st.dma("sp", grow[:], modv[l, ci, 5 * D:6 * D].partition_broadcast(128), writes=["grow"])
            As = [st.sb([128, 8, TB], BF16) for _ in range(3)]
            ws = [st.sb([128, 8, 512], BF16) for _ in range(3)]
            xt = [st.sb([128, 512], F32) for _ in range(3)]
            tt_ = [st.sb([128, 512], F32) for _ in range(3)]
            ntok = TB // 128
            it = 0
            xi = 0
            for cb in range(8):
                cs = slice(cb * 512, (cb + 1) * 512)
                for ex in range(NE):
                    s = it % 3
                    it += 1
                    st.dma("sp", As[s][:], aT[ex * DE:(ex + 1) * DE].rearrange("(k p) t -> p k t", p=128)[:, :, 0:TB], writes=[("A", s)])
                    st.dma("pool", ws[s][:], W["moe_w_down"][l, ex].rearrange("(k p) c -> p k c", p=128)[:, :, cs], writes=[("w", s)])
                    st.pe([(lambda e, tt=tt, k=k, s=s, ex=ex: e.matmul(bank(tt), lhsT=As[s][:, k, tt * 128:(tt + 1) * 128], rhs=ws[s][:, k, :],
                                                                      start=(ex == 0 and k == 0), stop=(ex == NE - 1 and k == 7)))
                           for tt in range(ntok) for k in range(8)], reads=[("A", s), ("w", s)], writes=[("ps", tt) for tt in range(ntok)])
                for tt in range(ntok):
                    q = xi % 3
                    xi += 1
                    rows = slice(tok0 + tt * 128, tok0 + (tt + 1) * 128)
                    st.dma("sp", xt[q][:], xres[rows, cs], reads=[("x", tt, cb)], writes=[("xt", q)])
                    st.op("dve", lambda e, q=q, tt=tt, cs=cs: e.tensor_tensor(out=tt_[q][:], in0=bank(tt), in1=grow[:, cs], op=ALU.mult),
                          reads=[("ps", tt), "grow"], writes=[("tt", q)])
                    st.op("dve", lambda e, q=q: e.tensor_tensor(out=tt_[q][:], in0=tt_[q][:], in1=xt[q][:], op=ALU.add),
                          reads=[("tt", q), ("xt", q)], writes=[("tt", q)])
                    st.dma("sp", xres[rows, cs], tt_[q][:], reads=[("tt", q)], writes=[("x", tt, cb)])
            st.flush()

        groups = [(0, LS, [(0, LS)], 0), (LS, LP, [(i * PSEQ, PSEQ) for i in range(NP)], 1)]
        plan = []
        plan.append(("ada", stage_ada, ()))
        for grp in groups:
            for l in range(depth):
                plan.append(("norm1", stage_norm, (l, 1, grp)))
                plan.append(("proj", stage_proj, (l, grp)))
                plan.append(("tr", stage_tr, (grp,)))
                plan.append(("ssd1", stage_ssd1, (l, grp)))
                plan.append(("ssd2", stage_ssd2, (l, grp)))
                plan.append(("o1", stage_o1, (l, grp)))
                plan.append(("o2", stage_o2, (l, grp)))
                plan.append(("wo", stage_wo, (l, grp)))
                plan.append(("norm2", stage_norm, (l, 2, grp)))
                plan.append(("gu", stage_gu, (l, grp)))
                plan.append(("down", stage_down, (l, grp)))
            plan.append(("final", stage_norm, (depth - 1, "f", grp)))
        for nm, fn, args in plan:
            if only is not None and nm not in only:
                continue
            fn(*args)
            if stop_after is not None and nm == stop_after:
                break
    return nc


def make_consts():
    c = np.zeros((128, 4, 128), np.float32)
    i = np.arange(128)
    c[:, 0, :] = np.eye(128, dtype=np.float32)
    c[:, 1, :] = (i[:, None] <= i[None, :]).astype(np.float32)
    c[:, 2, :] = (i[:, None] >= i[None, :]).astype(np.float32)
    c[:, 3, :] = 1.0
    return c


def core_inputs(inputs, core, ROWS=16, NP=4):
    LS = ROWS * GRID_W
    sb = core % 4
    xs = inputs["x_sample"][sb][:LS]
    xp = inputs["x_prompt"][core * NP:(core + 1) * NP].reshape(NP * PSEQ, D)
    m = {
        "xin": np.ascontiguousarray(np.concatenate([xs, xp], axis=0)),
        "cond": np.ascontiguousarray(np.stack([inputs["c"][sb], inputs["c_ctx"]], axis=0).reshape(2, KC, 128).transpose(2, 1, 0)),
        "h0s": np.ascontiguousarray(inputs["state_ssd"][sb].reshape(DEPTH, 2, D, 128)),
        "cst": make_consts(),
    }
    for nm in ["ada_w", "ada_b", "norm1_g", "norm2_g", "w_in", "ssd_d", "ssd_norm_g",
               "ssd_out", "sc_out", "cf_out",
               "w_o", "router_w", "router_bias", "moe_w_gate", "moe_w_up", "moe_w_down", "final_g"]:
        m[nm] = np.ascontiguousarray(inputs[nm])
    for nm in ["ssd_conv_w", "sc_conv_w", "cf_conv_w"]:
        a = inputs[nm]
        m[nm] = np.ascontiguousarray(a.reshape(a.shape[0], a.shape[1], -1, 128).transpose(0, 3, 2, 1))
    for nm in ["ssd_conv_b", "sc_conv_b", "cf_conv_b", "cf_ln_g", "cf_ln_b"]:
        a = inputs[nm]
        m[nm] = np.ascontiguousarray(a.reshape(a.shape[0], -1, 128).transpose(0, 2, 1))
    m["ssd_dt_bias"] = np.ascontiguousarray(inputs["ssd_dt_bias"].reshape(DEPTH, 128))
    m["ssd_a_log"] = np.ascontiguousarray(inputs["ssd_a_log"].reshape(DEPTH, 128))
    return m


def kernel(**inputs):
    inputs = {k: np.asarray(v) for k, v in inputs.items()}
    nc = build()
    in_maps = [core_inputs(inputs, cidx) for cidx in range(8)]
    res = run_bass_kernel_spmd(nc, in_maps, core_ids=list(range(8)))
    LS = 1024
    y_prompt = np.zeros((32, 256, D), np.float32)
    y_sample = np.zeros((4, 1024, D), np.float32)
    nstate = np.zeros((32, DEPTH, 2, 64, 64, 128), np.float32)
    for cidx in range(8):
        r = res.results[cidx]
        if cidx < 4:
            y_sample[cidx] = r["y"][:LS]
        y_prompt[cidx * 4:(cidx + 1) * 4] = r["y"][LS:].reshape(4, 256, D)
        nstate[cidx * 4:(cidx + 1) * 4] = r["nst"].reshape(4, DEPTH, 2, 64, 64, 128)
    return y_prompt, y_sample, nstate
```

```python
import numpy as np
from contextlib import ExitStack
from collections import defaultdict
import concourse.bass as bass
import concourse.mybir as mybir
from concourse.bass_utils import run_bass_kernel_spmd

F32 = mybir.dt.float32
BF16 = mybir.dt.bfloat16
AF = mybir.ActivationFunctionType
ALU = mybir.AluOpType
AX = mybir.AxisListType

D = 4096
KC = 32
DEPTH = 2
GRID_W = 64
OFF_Z = 0
OFF_XBC = 4096
OFF_DT = OFF_XBC + 6144
OFF_SC = OFF_DT + 128
OFF_CF = OFF_SC + 6144
OFF_GATE = OFF_CF + 4096
IN_COLS = OFF_GATE + 3 * D
NE = 16
DE = 1024
EPS = 1e-6
PSEQ = 256


class Stage:
    NS = 6

    _uid = [0]
    _sets = [None, None]
    _es = None

    def __init__(self, nc, name):
        self.nc = nc
        Stage._uid[0] += 1
        name = f"{name}{Stage._uid[0]}"
        self.name = name
        self.es = ExitStack()
        self.ops = {e: [] for e in ("pe", "act", "dve", "pool", "sp")}
        if Stage._sets[0] is None:
            cs = {e: Stage._es.enter_context(nc.semaphore(f"g_c{e}")) for e in ("pe", "act", "dve", "pool")}
            ds = {q: [Stage._es.enter_context(nc.semaphore(f"g_d{q}{i}")) for i in range(self.NS)]
                  for q in ("sp", "pool")}
            Stage._sets[0] = (cs, ds)
            Stage._cnt = ({e: 0 for e in cs}, {q: [0] * self.NS for q in ds})
        self.csem, self.dsem = Stage._sets[0]
        self.ccnt, self.dcnt = Stage._cnt
        self.drr = {q: 0 for q in self.dsem}
        self.lastw = {}
        self.readers = defaultdict(list)
        self.nsb = 0

    def sb(self, shape, dt, name=None):
        self.nsb += 1
        return self.es.enter_context(self.nc.sbuf_tensor(f"{self.name}_{name or 't'}{self.nsb}", list(shape), dt))

    def _deps(self, reads, writes):
        toks = []
        for k in reads:
            t = self.lastw.get(k)
            if t is not None:
                toks.append(t)
        for k in writes:
            t = self.lastw.get(k)
            if t is not None:
                toks.append(t)
            toks.extend(self.readers.get(k, ()))
        return toks

    def _commit(self, tok, reads, writes):
        for k in writes:
            self.lastw[k] = tok
            self.readers[k] = []
        for k in reads:
            self.readers[k].append(tok)

    def op(self, eng, fn, reads=(), writes=()):
        toks = self._deps(reads, writes)
        self.ccnt[eng] += 1
        tok = ("c", eng, self.ccnt[eng])
        self.ops[eng].append((toks, [fn], (self.csem[eng], 1)))
        self._commit(tok, reads, writes)

    def pe(self, fns, reads=(), writes=()):
        toks = self._deps(reads, writes)
        self.ccnt["pe"] += 1
        tok = ("c", "pe", self.ccnt["pe"])
        self.ops["pe"].append((toks, list(fns), (self.csem["pe"], 1)))
        self._commit(tok, reads, writes)

    def dma(self, q, out, in_, reads=(), writes=(), slow=False):
        toks = self._deps(reads, writes)
        s = self.drr[q] % self.NS
        self.drr[q] += 1
        prev = self.dcnt[q][s]
        if prev > 0:
            toks.append(("d", q, s, prev))
        self.dcnt[q][s] = prev + 16
        tok = ("d", q, s, prev + 16)
        if slow:
            fn = lambda e: e.dma_start(out=out, in_=in_, allow_slow_non_contiguous=True)
        else:
            fn = lambda e: e.dma_start(out=out, in_=in_)
        self.ops[q].append((toks, [fn], (self.dsem[q][s], 16)))
        self._commit(tok, reads, writes)

    def flush(self):
        nc = self.nc
        ops = self.ops

        def resolve(tok):
            if tok[0] == "c":
                return self.csem[tok[1]], tok[2], tok[1]
            return self.dsem[tok[1]][tok[2]], tok[3], None

        def emit(engname, e):
            waited = {}
            for toks, fns, (sem, inc) in ops[engname]:
                for tok in toks:
                    s, v, src = resolve(tok)
                    if engname == "pe" and src == "pe":
                        continue
                    key = id(s)
                    if waited.get(key, 0) >= v:
                        continue
                    waited[key] = v
                    e.wait_ge(s, v)
                ins = None
                for fn in fns:
                    ins = fn(e)
                ins.then_inc(sem, inc)
            if engname in self.dsem:
                for s in range(self.NS):
                    if self.dcnt[engname][s] > 0:
                        e.wait_ge(self.dsem[engname][s], self.dcnt[engname][s])

        with nc.Block() as block:
            if ops["sp"]:
                @block.sync
                def _(e):
                    emit("sp", e)
            if ops["pool"]:
                @block.gpsimd
                def _(e):
                    emit("pool", e)
            if ops["pe"]:
                @block.tensor
                def _(e):
                    emit("pe", e)
            if ops["act"]:
                @block.scalar
                def _(e):
                    emit("act", e)
            if ops["dve"]:
                @block.vector
                def _(e):
                    emit("dve", e)
        self.es.close()


def build(ROWS=16, NP=4, debug=False, depth=DEPTH, stop_after=None, only=None):
    LS = ROWS * GRID_W
    LP = NP * PSEQ
    T = LS + LP
    TBM = max(LS, LP)
    assert TBM <= 1024 and LS % 128 == 0
    nc = bass.Bass("TRN2", target_bir_lowering=False)
    Stage._uid[0] = 0
    Stage._sets = [None, None]
    Stage._es = ExitStack()

    def din(name, shape):
        return nc.dram_tensor(name, list(shape), F32, kind="ExternalInput").ap()

    skind = "ExternalOutput" if debug else "Internal"

    def scr(name, shape, dt):
        return nc.dram_tensor(name, list(shape), dt, kind=skind).ap()

    xin = din("xin", [T, D])
    cond = din("cond", [128, KC, 2])
    h0s = din("h0s", [DEPTH, 2, D, 128])
    cst = din("cst", [128, 4, 128])
    W = {}
    for nm, shp in [("ada_w", [DEPTH, D, 6 * D]), ("ada_b", [DEPTH, 6 * D]), ("norm1_g", [DEPTH, D]),
                    ("norm2_g", [DEPTH, D]), ("w_in", [DEPTH, D, IN_COLS]), ("ssd_conv_w", [DEPTH, 128, 48, 3]),
                    ("ssd_conv_b", [DEPTH, 128, 48]), ("ssd_dt_bias", [DEPTH, 128]), ("ssd_a_log", [DEPTH, 128]),
                    ("ssd_d", [DEPTH, 64]), ("ssd_norm_g", [DEPTH, D]), ("ssd_out", [DEPTH, D, D]),
                    ("sc_conv_w", [DEPTH, 128, 16, 3]), ("sc_conv_b", [DEPTH, 128, 16]), ("sc_out", [DEPTH, 2048, D]),
                    ("cf_conv_w", [DEPTH, 128, 16, 31]), ("cf_conv_b", [DEPTH, 128, 16]), ("cf_ln_g", [DEPTH, 128, 16]),
                    ("cf_ln_b", [DEPTH, 128, 16]), ("cf_out", [DEPTH, 2048, D]), ("w_o", [DEPTH, D, D]),
                    ("router_w", [D, NE]), ("router_bias", [NE]), ("moe_w_gate", [DEPTH, NE, D, DE]),
                    ("moe_w_up", [DEPTH, NE, D, DE]), ("moe_w_down", [DEPTH, NE, DE, D]), ("final_g", [D])]:
        W[nm] = din(nm, shp)
    y = nc.dram_tensor("y", [T, D], F32, kind="ExternalOutput").ap()
    nst = nc.dram_tensor("nst", [NP, DEPTH, 2, D, 128], F32, kind="ExternalOutput").ap()

    NCHM = TBM // 128
    modv = scr("modv", [DEPTH, 2, 6 * D], F32)
    hT = scr("hT", [D, TBM], BF16)
    zs = scr("zs", [TBM, D], BF16)
    xbcf = scr("xbcf", [6144, TBM], BF16)
    xbt = scr("xbt", [TBM, 5120], BF16)
    vsc = scr("vsc", [2048, TBM], BF16)
    ucf = scr("ucf", [2048, TBM], F32)
    gts = scr("gts", [3 * D, TBM], BF16)
    dts = scr("dts", [NCHM, 128, 5, 128], F32)
    acb = scr("acb", [NCHM, 2, 64, 128], F32)
    hent = scr("hent", [NCHM, 2, 128, D], BF16)
    ynf = scr("ynf", [D, TBM], BF16)
    mrg = scr("mrg", [D, TBM], BF16)
    xres = scr("xres", [T, D], F32)
    comb = scr("comb", [NE, TBM], F32)
    aT = scr("aT", [NE * DE, TBM], BF16)

    with nc.psum_tensor("PS", [128, 4096], F32) as PS:
        def bank(b, n=512):
            return PS[:, b * 512:b * 512 + n]

        def bankbf(b):
            return PS[:, b * 512:(b + 1) * 512].bitcast(BF16)

        def load_consts(st):
            c = st.sb([128, 4, 128], F32, "cst")
            st.dma("sp", c[:], cst, writes=["cst"])
            return c

        def stage_ada():
            st = Stage(nc, "ada")
            cT = st.sb([128, KC, 2], F32)
            cTb = st.sb([128, KC, 2], BF16)
            st.dma("sp", cT[:], cond, writes=["cT"])
            st.op("act", lambda e: e.activation(out=cTb[:], in_=cT[:], func=AF.Silu), reads=["cT"], writes=["cTb"])
            ws = [st.sb([128, KC, 512], BF16) for _ in range(2)]
            bs = [st.sb([2, 512], F32) for _ in range(2)]
            os_ = [st.sb([2, 512], F32) for _ in range(2)]
            it = 0
            for l in range(depth):
                wv = W["ada_w"][l].rearrange("(k p) c -> p k c", p=128)
                for cb in range(48):
                    s = it % 2
                    cs = slice(cb * 512, (cb + 1) * 512)
                    st.dma("pool", ws[s][:], wv[:, :, cs], writes=[("w", s)])
                    st.dma("sp", bs[s][:], W["ada_b"][l, cs].partition_broadcast(2), writes=[("b", s)])
                    pb = bank(it % 8)
                    st.pe([(lambda e, k=k, s=s, pb=pb: e.matmul(pb[0:2, :], lhsT=cTb[:, k, :], rhs=ws[s][:, k, :],
                                                                  start=(k == 0), stop=(k == KC - 1)))
                           for k in range(KC)], reads=["cTb", ("w", s)], writes=[("ps", it % 8)])
                    st.op("dve", lambda e, s=s, pb=pb: e.tensor_tensor(out=os_[s][:], in0=pb[0:2, :], in1=bs[s][:],
                                                                         op=ALU.add),
                          reads=[("ps", it % 8), ("b", s)], writes=[("o", s)])
                    st.dma("sp", modv[l, :, cs], os_[s][:], reads=[("o", s)])
                    it += 1
            st.flush()

        def stage_norm(l, which, grp):
            tok0, TB, seqs, ci = grp
            st = Stage(nc, f"n{which}")
            c = load_consts(st)
            epsb = st.sb([128, 1], F32)
            st.op("dve", lambda e: e.memset(epsb[:], EPS), writes=["epsb"])
            identb = st.sb([128, 128], BF16)
            st.op("dve", lambda e: e.tensor_copy(out=identb[:], in_=c[:, 0, :]), reads=["cst"], writes=["idb"])
            G = st.sb([128, D], F32)
            if which == "f":
                st.dma("sp", G[:], W["final_g"].partition_broadcast(128), writes=["G"])
                xsrc = xres
            else:
                S = st.sb([128, D], F32)
                SH = st.sb([128, D], F32)
                gname = "norm1_g" if which == 1 else "norm2_g"
                o = 0 if which == 1 else 3 * D
                st.dma("sp", G[:], W[gname][l].partition_broadcast(128), writes=["G"])
                st.dma("sp", S[:], modv[l, ci, o + D:o + 2 * D].partition_broadcast(128), writes=["S"])
                st.dma("sp", SH[:], modv[l, ci, o:o + D].partition_broadcast(128), writes=["SH"])
                st.op("dve", lambda e: e.scalar_tensor_tensor(out=G[:], in0=S[:], scalar=1.0, in1=G[:],
                                                              op0=ALU.add, op1=ALU.mult), reads=["S", "G"], writes=["G"])
                xsrc = xin if (l == 0 and which == 1) else xres
            xs = [st.sb([128, D], F32) for _ in range(2)]
            junk = st.sb([128, D], BF16)
            ss = [st.sb([128, 1], F32) for _ in range(2)]
            hb = [st.sb([128, D], BF16) for _ in range(2)]
            hTs = [st.sb([128, KC, 128], BF16) for _ in range(2)]
            for i in range(TB // 128):
                s = i % 2
                rows = slice(tok0 + i * 128, tok0 + (i + 1) * 128)
                st.dma("sp", xs[s][:], xsrc[rows, :], reads=[("x", i)], writes=[("xs", s)])
                st.op("dve", lambda e, s=s: e.memset(ss[s][:], 0.0), writes=[("ss", s)])
                st.op("act", lambda e, s=s: e.activation(out=junk[:], in_=xs[s][:], func=AF.Square,
                                                         accum_out=ss[s][:, 0:1]),
                      reads=[("xs", s)], writes=["junk", ("ss", s)])
                st.op("act", lambda e, s=s: e.activation(out=ss[s][:], in_=ss[s][:], func=AF.Sqrt, scale=1.0 / D, bias=epsb[:, 0:1]),
                      reads=[("ss", s), "epsb"], writes=[("ss", s)])
                st.op("dve", lambda e, s=s: e.reciprocal(out=ss[s][:], in_=ss[s][:]), reads=[("ss", s)], writes=[("ss", s)])
                st.op("dve", lambda e, s=s: e.scalar_tensor_tensor(out=xs[s][:], in0=xs[s][:], scalar=ss[s][:, 0:1],
                                                                   in1=G[:], op0=ALU.mult, op1=ALU.mult),
                      reads=[("xs", s), ("ss", s), "G"], writes=[("xs", s)])
                if which == "f":
                    st.dma("sp", y[rows, :], xs[s][:], reads=[("xs", s)])
                    continue
                st.op("dve", lambda e, s=s: e.tensor_tensor(out=hb[s][:], in0=xs[s][:], in1=SH[:], op=ALU.add),
                      reads=[("xs", s), "SH"], writes=[("hb", s)])
                for q in range(4):
                    b = (i * 4 + q) % 8
                    pb = bankbf(b)
                    st.pe([(lambda e, r=r, q=q, s=s, pb=pb: e.transpose(out=pb[:, r * 128:(r + 1) * 128],
                                                                          in_=hb[s][:, (q * 8 + r) * 128:(q * 8 + r + 1) * 128],
                                                                          identity=identb[:]))
                           for r in range(8)], reads=[("hb", s), "idb"], writes=[("ps", b)])
                    eng = "act" if q % 2 == 0 else "dve"
                    dst = hTs[s][:, q * 8:(q + 1) * 8, :].rearrange("p a b -> p (a b)")
                    if eng == "act":
                        st.op("act", lambda e, dst=dst, pb=pb: e.activation(out=dst, in_=pb, func=AF.Copy),
                              reads=[("ps", b)], writes=[("hTs", s, q)])
                    else:
                        st.op("dve", lambda e, dst=dst, pb=pb: e.tensor_copy(out=dst, in_=pb),
                              reads=[("ps", b)], writes=[("hTs", s, q)])
                st.dma("sp", hT.rearrange("(k p) t -> p k t", p=128)[:, :, i * 128:(i + 1) * 128], hTs[s][:],
                       reads=[("hTs", s, q) for q in range(4)], writes=[("hTd", i)])
            st.flush()

        def load_resident(st, src, kc, TB, key):
            r = st.sb([128, kc, TB], BF16, "res")
            st.dma("sp", r[:], src.rearrange("(k p) t -> p k t", p=128)[:, :, 0:TB], writes=[key])
            return r

        def ntile(TB):
            NT = min(512, TB)
            return NT, TB // NT

        def stage_proj(l, grp):
            tok0, TB, seqs, ci = grp
            is_s = (ci == 0)
            st = Stage(nc, "pj")
            c = load_consts(st)
            hTr = load_resident(st, hT, KC, TB, "hTr")
            NT, ntt = ntile(TB)
            win = W["w_in"][l].rearrange("(k p) c -> p k c", p=128)
            ws = [st.sb([128, KC, 512], BF16, "w") for _ in range(2)]
            wit = [0]
            pit = [0]

            def wblock(colranges):
                s = wit[0] % 2
                wit[0] += 1
                o = 0
                for (c0, n) in colranges:
                    st.dma("pool", ws[s][:, :, o:o + n], win[:, :, c0:c0 + n], writes=[("w", s, o // 128 + i) for i in range(n // 128)])
                    o += n
                return s

            def fm_mm(s, wo):
                b0 = (pit[0] * ntt) % 8
                pit[0] += 1
                keys = [("ps", b0 + n) for n in range(ntt)]
                fns = []
                for n in range(ntt):
                    for k in range(KC):
                        fns.append(lambda e, n=n, k=k, s=s, wo=wo, b0=b0: e.matmul(
                            bank(b0 + n, NT), lhsT=ws[s][:, k, wo * 128:(wo + 1) * 128],
                            rhs=hTr[:, k, n * NT:(n + 1) * NT], start=(k == 0), stop=(k == KC - 1)))
                st.pe(fns, reads=["hTr", ("w", s, wo)], writes=keys)
                return PS[:, b0 * 512:b0 * 512 + TB] if ntt > 1 or NT == 512 else PS[:, b0 * 512:b0 * 512 + TB], keys

            wdt = st.sb([128, KC, 128], BF16)
            st.dma("pool", wdt[:], win[:, :, OFF_DT:OFF_DT + 128], writes=["wdt"])
            dtb = st.sb([128, 128], F32)
            abc = st.sb([128, 128], F32)
            st.dma("sp", dtb[:], W["ssd_dt_bias"][l].partition_broadcast(128), writes=["dtb"])
            st.dma("sp", abc[:], W["ssd_a_log"][l].partition_broadcast(128), writes=["abc"])
            st.op("act", lambda e: e.activation(out=abc[:], in_=abc[:], func=AF.Exp), reads=["abc"], writes=["abc"])
            st.op("dve", lambda e: e.tensor_scalar(out=abc[:], in0=abc[:], scalar1=-1.0, scalar2=None, op0=ALU.mult),
                  reads=["abc"], writes=["abc"])
            Ds = [st.sb([128, 5, 128], F32) for _ in range(2)]
            tmp = [[st.sb([128, 128], F32) for _ in range(6)] for _ in range(2)]
            for cch in range(TB // 128):
                s = cch % 2
                B0 = s * 4
                P0, P1, P2, P3 = bank(B0, 128), bank(B0 + 1, 128), bank(B0 + 2, 128), bank(B0 + 3, 128)
                k0, k1, k2, k3 = [("ps", B0 + i) for i in range(4)]
                x1, ax, e1, l1, dtA, t2 = tmp[s]
                Dt = Ds[s]
                tk = lambda n: ("tmp", s, n)
                st.pe([(lambda e, k=k, cch=cch, P0=P0: e.matmul(P0, lhsT=hTr[:, k, cch * 128:(cch + 1) * 128],
                                                               rhs=wdt[:, k, :], start=(k == 0), stop=(k == KC - 1)))
                       for k in range(KC)], reads=["hTr", "wdt"], writes=[k0])
                st.op("dve", lambda e, x1=x1, P0=P0: e.tensor_tensor(out=x1[:], in0=P0, in1=dtb[:], op=ALU.add),
                      reads=[k0, "dtb"], writes=[tk(0)])
                st.op("act", lambda e, x1=x1, ax=ax: e.activation(out=ax[:], in_=x1[:], func=AF.Abs),
                      reads=[tk(0)], writes=[tk(1)])
                st.op("act", lambda e, ax=ax, e1=e1: e.activation(out=e1[:], in_=ax[:], func=AF.Exp, scale=-1.0),
                      reads=[tk(1)], writes=[tk(2)])
                st.op("act", lambda e, l1=l1, e1=e1: e.activation(out=l1[:], in_=e1[:], func=AF.Ln, bias=1.0),
                      reads=[tk(2)], writes=[tk(3)])
                st.op("dve", lambda e, Dt=Dt, x1=x1, l1=l1: e.scalar_tensor_tensor(out=Dt[:, 0, :], in0=x1[:], scalar=0.0,
                                                                                  in1=l1[:], op0=ALU.max, op1=ALU.add),
                      reads=[tk(0), tk(3)], writes=[("D", s, 0)])
                st.op("dve", lambda e, Dt=Dt, dtA=dtA: e.tensor_tensor(out=dtA[:], in0=Dt[:, 0, :], in1=abc[:], op=ALU.mult),
                      reads=[("D", s, 0), "abc"], writes=[tk(4)])
                st.pe([lambda e, P1=P1, dtA=dtA: e.matmul(P1[:, 0:64], lhsT=c[:, 1, :], rhs=dtA[:, 0:64], start=True, stop=True),
                       lambda e, P1=P1, dtA=dtA: e.matmul(P1[:, 64:128], lhsT=c[:, 2, :], rhs=dtA[:, 64:128], start=True, stop=True)],
                      reads=["cst", tk(4)], writes=[k1])
                st.pe([lambda e, P2=P2, dtA=dtA: e.matmul(P2[0:64, :], lhsT=dtA[:, 0:64], rhs=c[:, 1, :], start=True, stop=True),
                       lambda e, P2=P2, dtA=dtA: e.matmul(P2[64:128, :], lhsT=dtA[:, 64:128], rhs=c[:, 2, :], start=True, stop=True)],
                      reads=["cst", tk(4)], writes=[k2])
                st.pe([lambda e, P3=P3, dtA=dtA: e.matmul(P3, lhsT=c[:, 3, :], rhs=dtA[:], start=True, stop=True)],
                      reads=["cst", tk(4)], writes=[k3])
                st.op("act", lambda e, Dt=Dt, P1=P1: e.activation(out=Dt[:, 1, :], in_=P1, func=AF.Copy, scale=-1.0),
                      reads=[k1], writes=[("D", s, 1)])
                st.op("act", lambda e, Dt=Dt, P1=P1: e.activation(out=Dt[:, 2, :], in_=P1, func=AF.Exp),
                      reads=[k1], writes=[("D", s, 2)])
                st.op("dve", lambda e, Dt=Dt, P3=P3, t2=t2: e.tensor_tensor(out=t2[:], in0=P3, in1=Dt[:, 1, :], op=ALU.add),
                      reads=[k3, ("D", s, 1)], writes=[tk(5)])
                st.op("act", lambda e, t2=t2: e.activation(out=t2[:], in_=t2[:], func=AF.Exp), reads=[tk(5)], writes=[tk(5)])
                st.op("dve", lambda e, Dt=Dt, t2=t2: e.tensor_tensor(out=Dt[:, 3, :], in0=t2[:], in1=Dt[:, 0, :], op=ALU.mult),
                      reads=[tk(5), ("D", s, 0)], writes=[("D", s, 3)])
                st.op("act", lambda e, Dt=Dt, P3=P3: e.activation(out=Dt[:, 4, :], in_=P3, func=AF.Exp),
                      reads=[k3], writes=[("D", s, 4)])
                st.op("act", lambda e, ax=ax, P2=P2: e.activation(out=ax[:], in_=P2, func=AF.Copy), reads=[k2], writes=[tk(1)])
                st.dma("sp", dts[cch], Dt[:], reads=[("D", s, i) for i in range(5)])
                st.dma("sp", acb[cch].rearrange("d h t -> (d h) t"), ax[:], reads=[tk(1)])

            zo = [st.sb([128, 512], BF16) for _ in range(2)]
            zi = 0
            for cb in range(8):
                s = wblock([(OFF_Z + cb * 512, 512)])
                for tt in range(TB // 128):
                    b = pit[0] % 8
                    pit[0] += 1
                    st.pe([(lambda e, k=k, tt=tt, s=s, b=b: e.matmul(bank(b), lhsT=hTr[:, k, tt * 128:(tt + 1) * 128],
                                                                    rhs=ws[s][:, k, :], start=(k == 0), stop=(k == KC - 1)))
                           for k in range(KC)], reads=["hTr"] + [("w", s, i) for i in range(4)], writes=[("ps", b)])
                    z = zi % 2
                    zi += 1
                    st.op("act", lambda e, z=z, b=b: e.activation(out=zo[z][:], in_=bank(b), func=AF.Silu),
                          reads=[("ps", b)], writes=[("zo", z)])
                    st.dma("sp", zs[tt * 128:(tt + 1) * 128, cb * 512:(cb + 1) * 512], zo[z][:], reads=[("zo", z)])
            pit[0] = 0

            cw = st.sb([128, 48, 3], F32)
            cbi = st.sb([128, 48], F32)
            st.dma("sp", cw[:], W["ssd_conv_w"][l], writes=["cw"])
            st.dma("sp", cbi[:], W["ssd_conv_b"][l], writes=["cbi"])
            t0s = [st.sb([128, TB], F32) for _ in range(2)]
            accs = [st.sb([128, TB], F32) for _ in range(2)]
            obs = [st.sb([128, TB], BF16) for _ in range(2)]
            nseg = len(seqs)
            L = seqs[0][1]
            ei = [0]

            def conv3(t0, acc, w3, bias, j, seglen, rk, wk):
                v = lambda a: a[:].rearrange("p (s l) -> p s l", l=seglen)
                st.op("dve", lambda e: e.tensor_scalar(out=acc[:], in0=t0[:], scalar1=w3[:, j, 1:2], scalar2=bias[:, j:j + 1],
                                                       op0=ALU.mult, op1=ALU.add), reads=rk, writes=wk)
                st.op("dve", lambda e: e.scalar_tensor_tensor(out=v(acc)[:, :, 1:], in0=v(t0)[:, :, 0:seglen - 1],
                                                              scalar=w3[:, j, 0:1], in1=v(acc)[:, :, 1:],
                                                              op0=ALU.mult, op1=ALU.add), reads=rk + wk, writes=wk)
                st.op("dve", lambda e: e.scalar_tensor_tensor(out=v(acc)[:, :, 0:seglen - 1], in0=v(t0)[:, :, 1:],
                                                              scalar=w3[:, j, 2:3], in1=v(acc)[:, :, 0:seglen - 1],
                                                              op0=ALU.mult, op1=ALU.add), reads=rk + wk, writes=wk)

            for blk in range(12):
                s = wblock([(OFF_XBC + blk * 512, 512)])
                for wo in range(4):
                    j = blk * 4 + wo
                    ps, keys = fm_mm(s, wo)
                    q = ei[0] % 2
                    ei[0] += 1
                    st.op("act", lambda e, q=q, ps=ps: e.activation(out=t0s[q][:], in_=ps, func=AF.Copy),
                          reads=keys, writes=[("t0", q)])
                    conv3(t0s[q], accs[q], cw, cbi, j, L, ["cw", "cbi", ("t0", q)], [("acc", q)])
                    st.op("act", lambda e, q=q: e.activation(out=obs[q][:], in_=accs[q][:], func=AF.Silu),
                          reads=[("acc", q)], writes=[("ob", q)])
                    st.dma("sp", xbcf[j * 128:(j + 1) * 128, 0:TB], obs[q][:], reads=[("ob", q)])

            scw = st.sb([128, 16, 3], F32)
            scb = st.sb([128, 16], F32)
            st.dma("sp", scw[:], W["sc_conv_w"][l], writes=["scw"])
            st.dma("sp", scb[:], W["sc_conv_b"][l], writes=["scb"])
            bgs = [st.sb([128, TB], F32) for _ in range(2)]
            cgs = [st.sb([128, TB], F32) for _ in range(2)]
            sseg = GRID_W if is_s else PSEQ
            for j in range(16):
                s = wblock([(OFF_SC + j * 128, 128), (OFF_SC + 2048 + j * 128, 128), (OFF_SC + 4096 + j * 128, 128)])
                q = j % 2
                ps, keys = fm_mm(s, 0)
                st.op("act", lambda e, q=q, ps=ps: e.activation(out=bgs[q][:], in_=ps, func=AF.Copy), reads=keys, writes=[("bg", q)])
                ps, keys = fm_mm(s, 1)
                st.op("act", lambda e, q=q, ps=ps: e.activation(out=cgs[q][:], in_=ps, func=AF.Copy), reads=keys, writes=[("cg", q)])
                ps, keys = fm_mm(s, 2)
                st.op("dve", lambda e, q=q, ps=ps: e.tensor_tensor(out=t0s[q][:], in0=ps, in1=cgs[q][:], op=ALU.mult),
                      reads=keys + [("cg", q)], writes=[("t0", q)])
                conv3(t0s[q], accs[q], scw, scb, j, sseg, ["scw", "scb", ("t0", q)], [("acc", q)])
                st.op("dve", lambda e, q=q: e.tensor_tensor(out=obs[q][:], in0=accs[q][:], in1=bgs[q][:], op=ALU.mult),
                      reads=[("acc", q), ("bg", q)], writes=[("ob", q)])
                st.dma("sp", vsc[j * 128:(j + 1) * 128, 0:TB], obs[q][:], reads=[("ob", q)])

            cfw = st.sb([128, 16, 31], F32)
            cfb = st.sb([128, 16], F32)
            st.dma("sp", cfw[:], W["cf_conv_w"][l], writes=["cfw"])
            st.dma("sp", cfb[:], W["cf_conv_b"][l], writes=["cfb"])
            for j in range(16):
                s = wblock([(OFF_CF + j * 128, 128), (OFF_CF + 2048 + j * 128, 128)])
                q = j % 2
                ps, keys = fm_mm(s, 0)
                st.op("act", lambda e, q=q, ps=ps: e.activation(out=bgs[q][:], in_=ps, func=AF.Copy), reads=keys, writes=[("bg", q)])
                ps, keys = fm_mm(s, 1)
                st.op("act", lambda e, q=q, ps=ps: e.activation(out=cgs[q][:], in_=ps, func=AF.Sigmoid), reads=keys, writes=[("cg", q)])
                u = t0s[q]
                acc = accs[q]
                st.op("dve", lambda e, q=q, u=u: e.tensor_tensor(out=u[:], in0=bgs[q][:], in1=cgs[q][:], op=ALU.mult),
                      reads=[("bg", q), ("cg", q)], writes=[("t0", q)])
                rk = ["cfw", "cfb", ("t0", q)]
                wk = [("acc", q)]
                st.op("dve", lambda e, u=u, acc=acc, j=j: e.tensor_scalar(out=acc[:], in0=u[:], scalar1=cfw[:, j, 15:16],
                                                                        scalar2=cfb[:, j:j + 1], op0=ALU.mult, op1=ALU.add),
                      reads=rk, writes=wk)
                for d in range(-15, 16):
                    if d == 0:
                        continue
                    k = d + 15
                    if is_s:
                        if abs(d) >= ROWS:
                            continue
                        sh = GRID_W * abs(d)
                        if d > 0:
                            oa, ia = acc[:, 0:TB - sh], u[:, sh:TB]
                        else:
                            oa, ia = acc[:, sh:TB], u[:, 0:TB - sh]
                    else:
                        v = lambda a: a[:].rearrange("p (s l) -> p s l", l=PSEQ)
                        ad = abs(d)
                        if d > 0:
                            oa, ia = v(acc)[:, :, 0:PSEQ - ad], v(u)[:, :, ad:PSEQ]
                        else:
                            oa, ia = v(acc)[:, :, ad:PSEQ], v(u)[:, :, 0:PSEQ - ad]
                    st.op("dve", lambda e, oa=oa, ia=ia, j=j, k=k: e.scalar_tensor_tensor(
                        out=oa, in0=ia, scalar=cfw[:, j, k:k + 1], in1=oa, op0=ALU.mult, op1=ALU.add),
                          reads=rk + wk, writes=wk)
                st.dma("sp", ucf[j * 128:(j + 1) * 128, 0:TB], acc[:], reads=wk)

            for blk in range(24):
                s = wblock([(OFF_GATE + blk * 512, 512)])
                for wo in range(4):
                    j = blk * 4 + wo
                    ps, keys = fm_mm(s, wo)
                    q = ei[0] % 2
                    ei[0] += 1
                    st.op("act", lambda e, q=q, ps=ps: e.activation(out=obs[q][:], in_=ps, func=AF.Sigmoid),
                          reads=keys, writes=[("ob", q)])
                    st.dma("sp", gts[j * 128:(j + 1) * 128, 0:TB], obs[q][:], reads=[("ob", q)])
            st.flush()

        def stage_tr(grp):
            tok0, TB, seqs, ci = grp
            st = Stage(nc, "tr")
            c = load_consts(st)
            identb = st.sb([128, 128], BF16)
            st.op("dve", lambda e: e.tensor_copy(out=identb[:], in_=c[:, 0, :]), reads=["cst"], writes=["idb"])
            nti = TB // 128
            xs = [st.sb([128, TB], BF16) for _ in range(3)]
            ts = [st.sb([128, 8, 128], BF16) for _ in range(3)]
            xv = xbt.rearrange("(i p) c -> p i c", p=128)
            for j in range(40):
                s = j % 3
                b = j % 8
                st.dma("sp", xs[s][:], xbcf[j * 128:(j + 1) * 128, 0:TB], writes=[("xs", s)])
                pb = bankbf(b)
                st.pe([(lambda e, i=i, s=s, pb=pb: e.transpose(out=pb[:, i * 128:(i + 1) * 128],
                                                              in_=xs[s][:, i * 128:(i + 1) * 128], identity=identb[:]))
                       for i in range(nti)], reads=[("xs", s), "idb"], writes=[("ps", b)])
                dst = ts[s][:, 0:nti, :].rearrange("p a b -> p (a b)")
                if j % 2 == 0:
                    st.op("act", lambda e, dst=dst, pb=pb: e.activation(out=dst, in_=pb[:, 0:nti * 128], func=AF.Copy),
                          reads=[("ps", b)], writes=[("ts", s)])
                else:
                    st.op("dve", lambda e, dst=dst, pb=pb: e.tensor_copy(out=dst, in_=pb[:, 0:nti * 128]),
                          reads=[("ps", b)], writes=[("ts", s)])
                st.dma("sp", xv[:, 0:nti, j * 128:(j + 1) * 128], ts[s][:, 0:nti, :], reads=[("ts", s)])
            st.flush()

        def stage_ssd1(l, grp):
            tok0, TB, seqs, ci = grp
            is_s = (ci == 0)
            st = Stage(nc, "s1")
            c = load_consts(st)
            hst = [st.sb([128, D], F32, "hst") for _ in range(2)]
            h0t = st.sb([128, KC, 128], F32)
            snb = [st.sb([128, D], BF16) for _ in range(2)]
            xbs = [st.sb([128, 5120], BF16) for _ in range(2)]
            Dts = [st.sb([128, 5, 128], F32) for _ in range(2)]
            xsc = [st.sb([128, D], BF16) for _ in range(2)]
            it = 0
            pbi = 0
            for si, (s0, L) in enumerate(seqs):
                nch = L // 128
                c0 = s0 // 128
                for dr in range(2):
                    H = hst[dr]
                    hk = ("hst", dr)
                    if is_s:
                        st.dma("sp", h0t[:], h0s[l, dr].rearrange("(q p) n -> p q n", p=128), reads=[], writes=["h0t"])
                        for q4 in range(8):
                            b = pbi % 8
                            pbi += 1
                            st.pe([(lambda e, r=r, q4=q4, b=b: e.transpose(out=bank(b)[:, r * 128:(r + 1) * 128],
                                                                           in_=h0t[:, q4 * 4 + r, :], identity=c[:, 0, :]))
                                   for r in range(4)], reads=["h0t", "cst"], writes=[("ps", b)])
                            st.op("act", lambda e, H=H, q4=q4, b=b: e.activation(out=H[:, q4 * 512:(q4 + 1) * 512], in_=bank(b), func=AF.Copy),
                                  reads=[("ps", b)], writes=[hk])
                    else:
                        st.op("dve", lambda e, H=H: e.memset(H[:], 0.0), writes=[hk])
                    order = range(nch) if dr == 0 else range(nch - 1, -1, -1)
                    for cc in order:
                        cch = c0 + cc
                        s = it % 2
                        it += 1
                        st.op("act", lambda e, H=H, s=s: e.activation(out=snb[s][:], in_=H[:], func=AF.Copy),
                              reads=[hk], writes=[("snb", s)])
                        st.dma("sp", hent[cch, dr], snb[s][:], reads=[("snb", s)])
                        st.dma("sp", xbs[s][:], xbt[cch * 128:(cch + 1) * 128, :], writes=[("xb", s)])
                        st.dma("sp", Dts[s][:], dts[cch], writes=[("Dt", s)])
                        st.op("dve", lambda e, s=s, dr=dr: e.tensor_tensor(
                            out=xsc[s][:].rearrange("p (h q) -> p h q", q=64),
                            in0=xbs[s][:, 0:D].rearrange("p (h q) -> p h q", q=64),
                            in1=Dts[s][:, 3, dr * 64:(dr + 1) * 64].unsqueeze(2).broadcast_to([128, 64, 64]), op=ALU.mult),
                              reads=[("xb", s), ("Dt", s)], writes=[("xsc", s)])
                        for g in range(8):
                            b = pbi % 8
                            pbi += 1
                            st.pe([lambda e, s=s, g=g, b=b: e.matmul(bank(b), lhsT=xbs[s][:, D + g * 128:D + (g + 1) * 128],
                                                                  rhs=xsc[s][:, g * 512:(g + 1) * 512], start=True, stop=True)],
                                  reads=[("xb", s), ("xsc", s)], writes=[("ps", b)])
                            Hg = H[:, g * 512:(g + 1) * 512]
                            st.op("dve", lambda e, Hg=Hg, s=s, dr=dr, g=g: e.tensor_tensor(
                                out=Hg.rearrange("p (h q) -> p h q", q=64), in0=Hg.rearrange("p (h q) -> p h q", q=64),
                                in1=Dts[s][:, 4, dr * 64 + g * 8:dr * 64 + (g + 1) * 8].unsqueeze(2).broadcast_to([128, 8, 64]),
                                op=ALU.mult), reads=[hk, ("Dt", s)], writes=[hk])
                            st.op("dve", lambda e, Hg=Hg, b=b: e.tensor_tensor(out=Hg, in0=Hg, in1=bank(b), op=ALU.add),
                                  reads=[hk, ("ps", b)], writes=[hk])
                    if not is_s:
                        fo = h0t
                        for q4 in range(8):
                            b = pbi % 8
                            pbi += 1
                            st.pe([(lambda e, r=r, q4=q4, b=b, H=H: e.transpose(out=bank(b)[:, r * 128:(r + 1) * 128],
                                                                                in_=H[:, (q4 * 4 + r) * 128:(q4 * 4 + r + 1) * 128],
                                                                                identity=c[:, 0, :]))
                                   for r in range(4)], reads=[hk, "cst"], writes=[("ps", b)])
                            st.op("act", lambda e, q4=q4, b=b: e.activation(
                                out=fo[:, q4 * 4:(q4 + 1) * 4, :].rearrange("p a b -> p (a b)"), in_=bank(b), func=AF.Copy),
                                  reads=[("ps", b)], writes=["h0t"])
                        st.dma("sp", nst[si, l, dr].rearrange("(q p) n -> p q n", p=128), fo[:], reads=["h0t"])
            st.flush()

        def stage_ssd2(l, grp):
            tok0, TB, seqs, ci = grp
            st = Stage(nc, "s2")
            c = load_consts(st)
            epsb = st.sb([128, 1], F32)
            st.op("dve", lambda e: e.memset(epsb[:], EPS), writes=["epsb"])
            identb = st.sb([128, 128], BF16)
            st.op("dve", lambda e: e.tensor_copy(out=identb[:], in_=c[:, 0, :]), reads=["cst"], writes=["idb"])
            d64 = st.sb([128, 64], F32)
            Dbc = st.sb([128, D], F32)
            gnb = st.sb([128, D], F32)
            st.dma("sp", d64[:], W["ssd_d"][l].partition_broadcast(128), writes=["d64"])
            st.dma("sp", gnb[:], W["ssd_norm_g"][l].partition_broadcast(128), writes=["gnb"])
            st.op("dve", lambda e: e.tensor_copy(out=Dbc[:].rearrange("p (h q) -> p h q", q=64),
                                                 in_=d64[:].unsqueeze(2).broadcast_to([128, 64, 64])),
                  reads=["d64"], writes=["Dbc"])
            xb = st.sb([128, 5120], BF16)
            Dt = st.sb([128, 5, 128], F32)
            zsb = st.sb([128, D], BF16)
            Bf = st.sb([128, 8, 128], BF16)
            Cf = st.sb([128, 8, 128], BF16)
            he = [st.sb([128, D], BF16) for _ in range(2)]
            ysb = st.sb([128, D], F32)
            ynb = st.sb([128, D], BF16)
            ynT = st.sb([128, KC, 128], BF16)
            junk = st.sb([128, D], BF16)
            ss = st.sb([128, 1], F32)
            cbm = [[st.sb([128, 128], F32) for _ in range(2)] for _ in range(2)]
            acbc = [[st.sb([128, 8, 128], F32) for _ in range(2)] for _ in range(2)]
            NR = 32
            segs = [st.sb([128, 128], F32) for _ in range(NR)]
            Wts = [st.sb([128, 128], BF16) for _ in range(NR)]
            t1s = [st.sb([128, 512], F32) for _ in range(2)]
            t2s = [st.sb([128, 512], F32) for _ in range(2)]
            Bv = xbcf[4096:5120].rearrange("(g p) t -> p g t", p=128)
            Cv = xbcf[5120:6144].rearrange("(g p) t -> p g t", p=128)
            ri = 0
            gi = 0
            for cch in range(TB // 128):
                tsl = slice(cch * 128, (cch + 1) * 128)
                st.dma("sp", xb[:], xbt[tsl, :], writes=["xb"])
                st.dma("sp", Dt[:], dts[cch], writes=["Dt"])
                st.dma("sp", zsb[:], zs[tsl, :], writes=["zsb"])
                st.dma("sp", Bf[:], Bv[:, :, tsl], writes=["Bf"])
                st.dma("sp", Cf[:], Cv[:, :, tsl], writes=["Cf"])
                st.dma("sp", he[0][:], hent[cch, 0], writes=[("he", 0)])
                st.dma("sp", he[1][:], hent[cch, 1], writes=[("he", 1)])
                for g in range(8):
                    p = gi % 2
                    gi += 1
                    bCB, bY, bIf, bIb = p, 2 + p, 4 + p, 6 + p
                    CBp = bank(bCB, 128)
                    st.pe([lambda e, g=g, CBp=CBp: e.matmul(CBp, lhsT=Bf[:, g, :], rhs=Cf[:, g, :], start=True, stop=True)],
                          reads=["Bf", "Cf"], writes=[("ps", bCB)])
                    for dr in range(2):
                        st.op("dve", lambda e, p=p, dr=dr, CBp=CBp: e.tensor_tensor(out=cbm[p][dr][:], in0=CBp, in1=c[:, 1 + dr, :],
                                                                                    op=ALU.mult),
                              reads=[("ps", bCB), "cst"], writes=[("cbm", p, dr)])
                        st.dma("sp", acbc[p][dr][:].rearrange("p a b -> p (a b)"),
                               acb[cch, dr, g * 8:(g + 1) * 8, :].rearrange("a b -> (a b)").partition_broadcast(128),
                               writes=[("acbc", p, dr)])
                    items = [(hh, dr) for hh in range(8) for dr in range(2)]
                    rs = []
                    for (hh, dr) in items:
                        r = ri % NR
                        ri += 1
                        rs.append(r)
                        col = dr * 64 + g * 8 + hh
                        st.op("dve", lambda e, r=r, p=p, dr=dr, hh=hh, col=col: e.tensor_scalar(
                            out=segs[r][:], in0=acbc[p][dr][:, hh, :], scalar1=Dt[:, 1, col:col + 1], scalar2=0.0,
                            op0=ALU.add, op1=ALU.min), reads=[("acbc", p, dr), "Dt"], writes=[("seg", r)])
                    for (hh, dr), r in zip(items, rs):
                        st.op("act", lambda e, r=r: e.activation(out=segs[r][:], in_=segs[r][:], func=AF.Exp),
                              reads=[("seg", r)], writes=[("seg", r)])
                    for (hh, dr), r in zip(items, rs):
                        col = dr * 64 + g * 8 + hh
                        st.op("dve", lambda e, r=r, p=p, dr=dr, col=col: e.scalar_tensor_tensor(
                            out=Wts[r][:], in0=segs[r][:], scalar=Dt[:, 0, col:col + 1], in1=cbm[p][dr][:],
                            op0=ALU.mult, op1=ALU.mult), reads=[("seg", r), "Dt", ("cbm", p, dr)], writes=[("Wt", r)])
                    for (hh, dr), r in zip(items, rs):
                        head = g * 8 + hh
                        st.pe([lambda e, r=r, bY=bY, hh=hh, head=head, dr=dr: e.matmul(
                            bank(bY)[:, hh * 64:(hh + 1) * 64], lhsT=Wts[r][:], rhs=xb[:, head * 64:(head + 1) * 64],
                            start=(dr == 0), stop=(dr == 1))], reads=[("Wt", r), "xb"], writes=[("ps", bY)])
                    st.pe([lambda e, g=g, bIf=bIf: e.matmul(bank(bIf), lhsT=Cf[:, g, :], rhs=he[0][:, g * 512:(g + 1) * 512],
                                                         start=True, stop=True)], reads=["Cf", ("he", 0)], writes=[("ps", bIf)])
                    st.pe([lambda e, g=g, bIb=bIb: e.matmul(bank(bIb), lhsT=Cf[:, g, :], rhs=he[1][:, g * 512:(g + 1) * 512],
                                                         start=True, stop=True)], reads=["Cf", ("he", 1)], writes=[("ps", bIb)])
                    t1, t2 = t1s[p], t2s[p]
                    v3 = lambda a: a.rearrange("p (h q) -> p h q", q=64)
                    eb = lambda dr, g=g: Dt[:, 2, dr * 64 + g * 8:dr * 64 + (g + 1) * 8].unsqueeze(2).broadcast_to([128, 8, 64])
                    st.op("dve", lambda e, t1=t1, bIf=bIf, eb=eb: e.tensor_tensor(out=v3(t1[:]), in0=v3(bank(bIf)), in1=eb(0), op=ALU.mult),
                          reads=[("ps", bIf), "Dt"], writes=[("t1", p)])
                    st.op("dve", lambda e, t1=t1, bY=bY: e.tensor_tensor(out=t1[:], in0=t1[:], in1=bank(bY), op=ALU.add),
                          reads=[("ps", bY), ("t1", p)], writes=[("t1", p)])
                    st.op("dve", lambda e, t2=t2, bIb=bIb, eb=eb: e.tensor_tensor(out=v3(t2[:]), in0=v3(bank(bIb)), in1=eb(1), op=ALU.mult),
                          reads=[("ps", bIb), "Dt"], writes=[("t2", p)])
                    st.op("dve", lambda e, t1=t1, t2=t2: e.tensor_tensor(out=t1[:], in0=t1[:], in1=t2[:], op=ALU.add),
                          reads=[("t1", p), ("t2", p)], writes=[("t1", p)])
                    st.op("dve", lambda e, t2=t2, g=g: e.tensor_tensor(out=t2[:], in0=xb[:, g * 512:(g + 1) * 512],
                                                                       in1=Dbc[:, g * 512:(g + 1) * 512], op=ALU.mult),
                          reads=["xb", "Dbc", ("t2", p)], writes=[("t2", p)])
                    st.op("dve", lambda e, t1=t1, t2=t2, g=g: e.tensor_tensor(out=ysb[:, g * 512:(g + 1) * 512], in0=t1[:], in1=t2[:],
                                                                              op=ALU.add),
                          reads=[("t1", p), ("t2", p)], writes=["ysb"])
                st.op("dve", lambda e: e.tensor_tensor(out=ysb[:], in0=ysb[:], in1=zsb[:], op=ALU.mult),
                      reads=["ysb", "zsb"], writes=["ysb"])
                st.op("dve", lambda e: e.memset(ss[:], 0.0), writes=["ss"])
                st.op("act", lambda e: e.activation(out=junk[:], in_=ysb[:], func=AF.Square, accum_out=ss[:, 0:1]),
                      reads=["ysb", "ss"], writes=["junk", "ss"])
                st.op("act", lambda e: e.activation(out=ss[:], in_=ss[:], func=AF.Sqrt, scale=1.0 / D, bias=epsb[:, 0:1]),
                      reads=["ss", "epsb"], writes=["ss"])
                st.op("dve", lambda e: e.reciprocal(out=ss[:], in_=ss[:]), reads=["ss"], writes=["ss"])
                st.op("dve", lambda e: e.scalar_tensor_tensor(out=ynb[:], in0=ysb[:], scalar=ss[:, 0:1], in1=gnb[:],
                                                              op0=ALU.mult, op1=ALU.mult), reads=["ysb", "ss", "gnb"], writes=["ynb"])
                for q in range(4):
                    b = q
                    pb = bankbf(b)
                    st.pe([(lambda e, r=r, q=q, pb=pb: e.transpose(out=pb[:, r * 128:(r + 1) * 128],
                                                                  in_=ynb[:, (q * 8 + r) * 128:(q * 8 + r + 1) * 128], identity=identb[:]))
                           for r in range(8)], reads=["ynb", "idb"], writes=[("ps", b)])
                    dst = ynT[:, q * 8:(q + 1) * 8, :].rearrange("p a b -> p (a b)")
                    st.op("act", lambda e, dst=dst, pb=pb: e.activation(out=dst, in_=pb, func=AF.Copy),
                          reads=[("ps", b)], writes=[("ynT", q)])
                st.dma("sp", ynf.rearrange("(k p) t -> p k t", p=128)[:, :, tsl], ynT[:], reads=[("ynT", q) for q in range(4)])
            st.flush()

        def stage_o1(l, grp):
            tok0, TB, seqs, ci = grp
            st = Stage(nc, "o1")
            yr = load_resident(st, ynf, KC, TB, "yr")
            NT, ntt = ntile(TB)
            wv = W["ssd_out"][l].rearrange("(k p) c -> p k c", p=128)
            ws = [st.sb([128, KC, 512], BF16) for _ in range(2)]
            gt = [st.sb([128, TB], BF16) for _ in range(2)]
            ob = [st.sb([128, TB], BF16) for _ in range(2)]
            pit = 0
            for blk in range(8):
                s = blk % 2
                st.dma("pool", ws[s][:], wv[:, :, blk * 512:(blk + 1) * 512], writes=[("w", s)])
                for wo in range(4):
                    j = blk * 4 + wo
                    b0 = (pit * ntt) % 8
                    pit += 1
                    q = j % 2
                    keys = [("ps", b0 + n) for n in range(ntt)]
                    st.pe([(lambda e, n=n, k=k, s=s, wo=wo, b0=b0: e.matmul(bank(b0 + n, NT), lhsT=ws[s][:, k, wo * 128:(wo + 1) * 128],
                                                                           rhs=yr[:, k, n * NT:(n + 1) * NT], start=(k == 0), stop=(k == KC - 1)))
                           for n in range(ntt) for k in range(KC)], reads=["yr", ("w", s)], writes=keys)
                    st.dma("sp", gt[q][:], gts[j * 128:(j + 1) * 128, 0:TB], writes=[("gt", q)])
                    st.op("dve", lambda e, q=q, b0=b0: e.tensor_tensor(out=ob[q][:], in0=PS[:, b0 * 512:b0 * 512 + TB], in1=gt[q][:], op=ALU.mult),
                          reads=keys + [("gt", q)], writes=[("ob", q)])
                    st.dma("sp", mrg[j * 128:(j + 1) * 128, 0:TB], ob[q][:], reads=[("ob", q)])
            st.flush()

        def stage_o2(l, grp):
            tok0, TB, seqs, ci = grp
            st = Stage(nc, "o2")
            c = load_consts(st)
            epsb = st.sb([128, 1], F32)
            st.op("dve", lambda e: e.memset(epsb[:], EPS), writes=["epsb"])
            vr = load_resident(st, vsc, 16, TB, "vr")
            ur = st.sb([128, 16, TB], BF16)
            lg = st.sb([128, 16], F32)
            lb = st.sb([128, 16], F32)
            st.dma("sp", lg[:], W["cf_ln_g"][l], writes=["lg"])
            st.dma("sp", lb[:], W["cf_ln_b"][l], writes=["lb"])
            LT = min(256, TB)
            uin = st.sb([128, 16, LT], F32)
            usq = st.sb([128, 16, LT], F32)
            mean = st.sb([128, LT], F32)
            msq = st.sb([128, LT], F32)
            rstd = st.sb([128, LT], F32)
            tt_ = [st.sb([128, LT], F32) for _ in range(2)]
            uv = ucf.rearrange("(j p) t -> p j t", p=128)
            for ti in range(TB // LT):
                tsl = slice(ti * LT, (ti + 1) * LT)
                st.dma("sp", uin[:], uv[:, :, tsl], writes=["uin"])
                st.op("dve", lambda e: e.tensor_tensor(out=usq[:], in0=uin[:], in1=uin[:], op=ALU.mult), reads=["uin"], writes=["usq"])
                bm, be = bank(0, LT), bank(1, LT)
                st.pe([(lambda e, j=j, bm=bm: e.matmul(bm, lhsT=c[:, 3, :], rhs=uin[:, j, :], start=(j == 0), stop=(j == 15)))
                       for j in range(16)], reads=["uin", "cst"], writes=[("ps", 0)])
                st.pe([(lambda e, j=j, be=be: e.matmul(be, lhsT=c[:, 3, :], rhs=usq[:, j, :], start=(j == 0), stop=(j == 15)))
                       for j in range(16)], reads=["usq", "cst"], writes=[("ps", 1)])
                st.op("dve", lambda e, bm=bm: e.tensor_scalar(out=mean[:], in0=bm, scalar1=1.0 / 2048, scalar2=None, op0=ALU.mult),
                      reads=[("ps", 0)], writes=["mean"])
                st.op("dve", lambda e: e.tensor_tensor(out=msq[:], in0=mean[:], in1=mean[:], op=ALU.mult), reads=["mean"], writes=["msq"])
                st.op("dve", lambda e, be=be: e.scalar_tensor_tensor(out=rstd[:], in0=be, scalar=1.0 / 2048, in1=msq[:],
                                                                    op0=ALU.mult, op1=ALU.subtract), reads=[("ps", 1), "msq"], writes=["rstd"])
                st.op("act", lambda e: e.activation(out=rstd[:], in_=rstd[:], func=AF.Sqrt, bias=epsb[:, 0:1]),
                      reads=["rstd", "epsb"], writes=["rstd"])
                st.op("dve", lambda e: e.reciprocal(out=rstd[:], in_=rstd[:]), reads=["rstd"], writes=["rstd"])
                for j in range(16):
                    t = tt_[j % 2]
                    st.op("dve", lambda e, t=t, j=j: e.tensor_tensor(out=t[:], in0=uin[:, j, :], in1=mean[:], op=ALU.subtract),
                          reads=["uin", "mean"], writes=[("t", j % 2)])
                    st.op("dve", lambda e, t=t: e.tensor_tensor(out=t[:], in0=t[:], in1=rstd[:], op=ALU.mult),
                          reads=[("t", j % 2), "rstd"], writes=[("t", j % 2)])
                    st.op("act", lambda e, t=t, j=j, tsl=tsl: e.activation(out=ur[:, j, tsl], in_=t[:], func=AF.Silu,
                                                                         scale=lg[:, j:j + 1], bias=lb[:, j:j + 1]),
                          reads=[("t", j % 2), "lg", "lb"], writes=["ur"])
            NT, ntt = ntile(TB)
            sv = W["sc_out"][l].rearrange("(k p) c -> p k c", p=128)
            cv = W["cf_out"][l].rearrange("(k p) c -> p k c", p=128)
            ws = [st.sb([128, KC, 256], BF16) for _ in range(2)]
            g1 = [st.sb([128, TB], BF16) for _ in range(2)]
            g2 = [st.sb([128, TB], BF16) for _ in range(2)]
            mt = [st.sb([128, TB], BF16) for _ in range(2)]
            aa = [st.sb([128, TB], F32) for _ in range(2)]
            bb = [st.sb([128, TB], F32) for _ in range(2)]
            ob = [st.sb([128, TB], BF16) for _ in range(2)]
            pit = 0
            for blk in range(16):
                s = blk % 2
                cs = slice(blk * 256, (blk + 1) * 256)
                st.dma("pool", ws[s][:, 0:16, :], sv[:, :, cs], writes=[("wa", s)])
                st.dma("pool", ws[s][:, 16:32, :], cv[:, :, cs], writes=[("wb", s)])
                for wo in range(2):
                    j = blk * 2 + wo
                    q = j % 2
                    b0 = (pit * 2 * ntt) % 8
                    pit += 1
                    ka = [("ps", b0 + n) for n in range(ntt)]
                    kb = [("ps", b0 + ntt + n) for n in range(ntt)]
                    st.pe([(lambda e, n=n, k=k, s=s, wo=wo, b0=b0: e.matmul(bank(b0 + n, NT), lhsT=ws[s][:, k, wo * 128:(wo + 1) * 128],
                                                                           rhs=vr[:, k, n * NT:(n + 1) * NT], start=(k == 0), stop=(k == 15)))
                           for n in range(ntt) for k in range(16)], reads=["vr", ("wa", s)], writes=ka)
                    st.pe([(lambda e, n=n, k=k, s=s, wo=wo, b0=b0: e.matmul(bank(b0 + ntt + n, NT), lhsT=ws[s][:, 16 + k, wo * 128:(wo + 1) * 128],
                                                                           rhs=ur[:, k, n * NT:(n + 1) * NT], start=(k == 0), stop=(k == 15)))
                           for n in range(ntt) for k in range(16)], reads=["ur", ("wb", s)], writes=kb)
                    rows = slice(j * 128, (j + 1) * 128)
                    st.dma("sp", g1[q][:], gts[D + j * 128:D + (j + 1) * 128, 0:TB], writes=[("g1", q)])
                    st.dma("sp", g2[q][:], gts[2 * D + j * 128:2 * D + (j + 1) * 128, 0:TB], writes=[("g2", q)])
                    st.dma("sp", mt[q][:], mrg[rows, 0:TB], reads=[("mrg", j)], writes=[("mt", q)])
                    pa = PS[:, b0 * 512:b0 * 512 + TB]
                    pb = PS[:, (b0 + ntt) * 512:(b0 + ntt) * 512 + TB]
                    st.op("dve", lambda e, q=q, pa=pa: e.tensor_tensor(out=aa[q][:], in0=pa, in1=g1[q][:], op=ALU.mult),
                          reads=ka + [("g1", q)], writes=[("aa", q)])
                    st.op("dve", lambda e, q=q, pb=pb: e.tensor_tensor(out=bb[q][:], in0=pb, in1=g2[q][:], op=ALU.mult),
                          reads=kb + [("g2", q)], writes=[("bb", q)])
                    st.op("dve", lambda e, q=q: e.tensor_tensor(out=aa[q][:], in0=aa[q][:], in1=bb[q][:], op=ALU.add),
                          reads=[("aa", q), ("bb", q)], writes=[("aa", q)])
                    st.op("dve", lambda e, q=q: e.tensor_tensor(out=ob[q][:], in0=aa[q][:], in1=mt[q][:], op=ALU.add),
                          reads=[("aa", q), ("mt", q)], writes=[("ob", q)])
                    st.dma("sp", mrg[rows, 0:TB], ob[q][:], reads=[("ob", q)], writes=[("mrg", j)])
            st.flush()

        def stage_wo(l, grp):
            tok0, TB, seqs, ci = grp
            st = Stage(nc, "wo")
            mr = load_resident(st, mrg, KC, TB, "mr")
            grow = st.sb([128, D], F32)
            st.dma("sp", grow[:], modv[l, ci, 2 * D:3 * D].partition_broadcast(128), writes=["grow"])
            xsrc = xin if l == 0 else xres
            wv = W["w_o"][l].rearrange("(k p) c -> p k c", p=128)
            ws = [st.sb([128, KC, 512], BF16) for _ in range(2)]
            xt = [st.sb([128, 512], F32) for _ in range(3)]
            tt_ = [st.sb([128, 512], F32) for _ in range(3)]
            it = 0
            for cb in range(8):
                s = cb % 2
                cs = slice(cb * 512, (cb + 1) * 512)
                st.dma("pool", ws[s][:], wv[:, :, cs], writes=[("w", s)])
                for tt in range(TB // 128):
                    b = it % 8
                    q = it % 3
                    it += 1
                    rows = slice(tok0 + tt * 128, tok0 + (tt + 1) * 128)
                    st.pe([(lambda e, k=k, tt=tt, s=s, b=b: e.matmul(bank(b), lhsT=mr[:, k, tt * 128:(tt + 1) * 128], rhs=ws[s][:, k, :],
                                                                    start=(k == 0), stop=(k == KC - 1))) for k in range(KC)],
                          reads=["mr", ("w", s)], writes=[("ps", b)])
                    st.dma("sp", xt[q][:], xsrc[rows, cs], reads=[("x", tt, cb)], writes=[("xt", q)])
                    st.op("dve", lambda e, q=q, b=b, cs=cs: e.tensor_tensor(out=tt_[q][:], in0=bank(b), in1=grow[:, cs], op=ALU.mult),
                          reads=[("ps", b), "grow"], writes=[("tt", q)])
                    st.op("dve", lambda e, q=q: e.tensor_tensor(out=tt_[q][:], in0=tt_[q][:], in1=xt[q][:], op=ALU.add),
                          reads=[("tt", q), ("xt", q)], writes=[("tt", q)])
                    st.dma("sp", xres[rows, cs], tt_[q][:], reads=[("tt", q)], writes=[("x", tt, cb)])
            st.flush()

        def stage_gu(l, grp):
            tok0, TB, seqs, ci = grp
            st = Stage(nc, "gu")
            c = load_consts(st)
            hTr = load_resident(st, hT, KC, TB, "hTr")
            rw = st.sb([128, KC, NE], BF16)
            st.dma("pool", rw[:], W["router_w"].rearrange("(k p) e -> p k e", p=128), writes=["rw"])
            rb = st.sb([128, NE], F32)
            st.dma("sp", rb[:], W["router_bias"].partition_broadcast(128), writes=["rb"])
            combT = st.sb([NE, TB], F32)
            sm = [[st.sb([128, NE], F32) for _ in range(6)] for _ in range(2)]
            s4 = [[st.sb([128, 4], F32) for _ in range(10)] for _ in range(2)]
            s1 = [[st.sb([128, 1], F32) for _ in range(3)] for _ in range(2)]
            for tt in range(TB // 128):
                p = tt % 2
                b = p
                sc_, sel, ge16, m16, w_, cmb = sm[p]
                mab, nab, mcd, ncd, m1, tmn, umx, m2, gs, gmask = s4[p]
                gmax, thr, wsum = s1[p]
                K = lambda n: ("r", p, n)
                st.pe([(lambda e, k=k, tt=tt, b=b: e.matmul(bank(b)[:, 0:NE], lhsT=hTr[:, k, tt * 128:(tt + 1) * 128], rhs=rw[:, k, :],
                                                           start=(k == 0), stop=(k == KC - 1))) for k in range(KC)],
                      reads=["hTr", "rw"], writes=[("ps", b)])
                st.op("act", lambda e, sc_=sc_, b=b: e.activation(out=sc_[:], in_=bank(b)[:, 0:NE], func=AF.Sigmoid),
                      reads=[("ps", b)], writes=[K("sc")])
                st.op("dve", lambda e, sel=sel, sc_=sc_: e.tensor_tensor(out=sel[:], in0=sc_[:], in1=rb[:], op=ALU.add),
                      reads=[K("sc"), "rb"], writes=[K("sel")])
                v = lambda a, i: a[:].rearrange("p (g x) -> p g x", x=4)[:, :, i]
                def tt2(out, a, b_, op, rk, wkk):
                    st.op("dve", lambda e: e.tensor_tensor(out=out, in0=a, in1=b_, op=op), reads=rk, writes=wkk)
                tt2(mab[:], v(sel, 0), v(sel, 1), ALU.max, [K("sel")], [K("mab")])
                tt2(nab[:], v(sel, 0), v(sel, 1), ALU.min, [K("sel")], [K("nab")])
                tt2(mcd[:], v(sel, 2), v(sel, 3), ALU.max, [K("sel")], [K("mcd")])
                tt2(ncd[:], v(sel, 2), v(sel, 3), ALU.min, [K("sel")], [K("ncd")])
                tt2(m1[:], mab[:], mcd[:], ALU.max, [K("mab"), K("mcd")], [K("m1")])
                tt2(tmn[:], mab[:], mcd[:], ALU.min, [K("mab"), K("mcd")], [K("tmn")])
                tt2(umx[:], nab[:], ncd[:], ALU.max, [K("nab"), K("ncd")], [K("umx")])
                tt2(m2[:], tmn[:], umx[:], ALU.max, [K("tmn"), K("umx")], [K("m2")])
                tt2(gs[:], m1[:], m2[:], ALU.add, [K("m1"), K("m2")], [K("gs")])
                st.op("dve", lambda e, gmax=gmax, gs=gs: e.tensor_reduce(out=gmax[:], in_=gs[:], axis=AX.X, op=ALU.max),
                      reads=[K("gs")], writes=[K("gmax")])
                st.op("dve", lambda e, gmask=gmask, gs=gs, gmax=gmax: e.tensor_scalar(out=gmask[:], in0=gs[:], scalar1=gmax[:, 0:1],
                                                                                  scalar2=None, op0=ALU.is_ge),
                      reads=[K("gs"), K("gmax")], writes=[K("gmask")])
                tt2(tmn[:], gmask[:], m2[:], ALU.mult, [K("gmask"), K("m2")], [K("tmn")])
                st.op("dve", lambda e, thr=thr, tmn=tmn: e.tensor_reduce(out=thr[:], in_=tmn[:], axis=AX.X, op=ALU.add),
                      reads=[K("tmn")], writes=[K("thr")])
                st.op("dve", lambda e, ge16=ge16, sel=sel, thr=thr: e.tensor_scalar(out=ge16[:], in0=sel[:], scalar1=thr[:, 0:1],
                                                                                scalar2=None, op0=ALU.is_ge),
                      reads=[K("sel"), K("thr")], writes=[K("ge16")])
                st.op("dve", lambda e, m16=m16, ge16=ge16, gmask=gmask: e.tensor_tensor(
                    out=m16[:].rearrange("p (g x) -> p g x", x=4), in0=ge16[:].rearrange("p (g x) -> p g x", x=4),
                    in1=gmask[:].unsqueeze(2).broadcast_to([128, 4, 4]), op=ALU.mult),
                      reads=[K("ge16"), K("gmask")], writes=[K("m16")])
                tt2(w_[:], sc_[:], m16[:], ALU.mult, [K("sc"), K("m16")], [K("w")])
                st.op("dve", lambda e, wsum=wsum, w_=w_: e.tensor_reduce(out=wsum[:], in_=w_[:], axis=AX.X, op=ALU.add),
                      reads=[K("w")], writes=[K("wsum")])
                st.op("dve", lambda e, wsum=wsum: e.reciprocal(out=wsum[:], in_=wsum[:]), reads=[K("wsum")], writes=[K("wsum")])
                st.op("dve", lambda e, cmb=cmb, w_=w_, wsum=wsum: e.tensor_scalar(out=cmb[:], in0=w_[:], scalar1=wsum[:, 0:1],
                                                                              scalar2=None, op0=ALU.mult),
                      reads=[K("w"), K("wsum")], writes=[K("cmb")])
                b2 = 2 + p
                st.pe([lambda e, cmb=cmb, b2=b2: e.transpose(out=bank(b2)[0:NE, 0:128], in_=cmb[:], identity=c[:, 0, :])],
                      reads=[K("cmb"), "cst"], writes=[("ps", b2)])
                st.op("act", lambda e, tt=tt, b2=b2: e.activation(out=combT[:, tt * 128:(tt + 1) * 128], in_=bank(b2)[0:NE, 0:128],
                                                                 func=AF.Copy), reads=[("ps", b2)], writes=["combT"])
            st.dma("sp", comb[:, 0:TB], combT[:], reads=["combT"], writes=["combd"])
            NT, ntt = ntile(TB)
            ws = [st.sb([128, KC, 512], BF16) for _ in range(2)]
            cbc = [st.sb([128, TB], F32) for _ in range(2)]
            sg = [st.sb([128, TB], F32) for _ in range(2)]
            ab = [st.sb([128, TB], BF16) for _ in range(2)]
            wi = 0
            pit = 0
            for ex in range(NE):
                ce = ex % 2
                st.dma("sp", cbc[ce][:], comb[ex, 0:TB].partition_broadcast(128), reads=["combd"], writes=[("cbc", ce)])
                gv = W["moe_w_gate"][l, ex].rearrange("(k p) c -> p k c", p=128)
                uv = W["moe_w_up"][l, ex].rearrange("(k p) c -> p k c", p=128)
                for jj in range(4):
                    s = wi % 2
                    wi += 1
                    st.dma("pool", ws[s][:, :, 0:256], gv[:, :, jj * 256:(jj + 1) * 256], writes=[("wg", s)])
                    st.dma("pool", ws[s][:, :, 256:512], uv[:, :, jj * 256:(jj + 1) * 256], writes=[("wu", s)])
                    for sub in range(2):
                        j = jj * 2 + sub
                        q = j % 2
                        b0 = (pit * 2 * ntt) % 8
                        pit += 1
                        kg = [("ps", b0 + n) for n in range(ntt)]
                        ku = [("ps", b0 + ntt + n) for n in range(ntt)]
                        st.pe([(lambda e, n=n, k=k, s=s, sub=sub, b0=b0: e.matmul(bank(b0 + n, NT), lhsT=ws[s][:, k, sub * 128:(sub + 1) * 128],
                                                                                rhs=hTr[:, k, n * NT:(n + 1) * NT], start=(k == 0), stop=(k == KC - 1)))
                               for n in range(ntt) for k in range(KC)], reads=["hTr", ("wg", s)], writes=kg)
                        st.pe([(lambda e, n=n, k=k, s=s, sub=sub, b0=b0: e.matmul(bank(b0 + ntt + n, NT), lhsT=ws[s][:, k, 256 + sub * 128:256 + (sub + 1) * 128],
                                                                                rhs=hTr[:, k, n * NT:(n + 1) * NT], start=(k == 0), stop=(k == KC - 1)))
                               for n in range(ntt) for k in range(KC)], reads=["hTr", ("wu", s)], writes=ku)
                        pg = PS[:, b0 * 512:b0 * 512 + TB]
                        pu = PS[:, (b0 + ntt) * 512:(b0 + ntt) * 512 + TB]
                        st.op("act", lambda e, q=q, pg=pg: e.activation(out=sg[q][:], in_=pg, func=AF.Silu), reads=kg, writes=[("sg", q)])
                        st.op("dve", lambda e, q=q, pu=pu: e.tensor_tensor(out=sg[q][:], in0=sg[q][:], in1=pu, op=ALU.mult),
                              reads=ku + [("sg", q)], writes=[("sg", q)])
                        st.op("dve", lambda e, q=q, ce=ce: e.tensor_tensor(out=ab[q][:], in0=sg[q][:], in1=cbc[ce][:], op=ALU.mult),
                              reads=[("sg", q), ("cbc", ce)], writes=[("ab", q)])
                        st.dma("sp", aT[ex * DE + j * 128:ex * DE + (j + 1) * 128, 0:TB], ab[q][:], reads=[("ab", q)])
            st.flush()

        def stage_down(l, grp):
            tok0, TB, seqs, ci = grp
            st = Stage(nc, "dn")
            grow = st.sb([128, D], F32)
            st.dma("sp", grow[:], modv[l, ci, 5 * D:6 * D].partition_broadcast(128), writes=["grow"])
            As = [st.sb([128, 8, TB], BF16) for _ in range(5)]
            ws = [st.sb([128, 8, 512], BF16) for _ in range(5)]
            xt = [st.sb([128, 512], F32) for _ in range(3)]
            tt_ = [st.sb([128, 512], F32) for _ in range(3)]
            ntok = TB // 128
            it = 0
            xi = 0
            for cb in range(8):
                cs = slice(cb * 512, (cb + 1) * 512)
                for ex in range(NE):
                    s = it % 5
                    it += 1
                    st.dma("sp", As[s][:], aT[ex * DE:(ex + 1) * DE].rearrange("(k p) t -> p k t", p=128)[:, :, 0:TB], writes=[("A", s)])
                    st.dma("pool", ws[s][:], W["moe_w_down"][l, ex].rearrange("(k p) c -> p k c", p=128)[:, :, cs], writes=[("w", s)])
                    st.pe([(lambda e, tt=tt, k=k, s=s, ex=ex: e.matmul(bank(tt), lhsT=As[s][:, k, tt * 128:(tt + 1) * 128], rhs=ws[s][:, k, :],
                                                                      start=(ex == 0 and k == 0), stop=(ex == NE - 1 and k == 7)))
                           for tt in range(ntok) for k in range(8)], reads=[("A", s), ("w", s)], writes=[("ps", tt) for tt in range(ntok)])
                for tt in range(ntok):
                    q = xi % 3
                    xi += 1
                    rows = slice(tok0 + tt * 128, tok0 + (tt + 1) * 128)
                    st.dma("sp", xt[q][:], xres[rows, cs], reads=[("x", tt, cb)], writes=[("xt", q)])
                    st.op("dve", lambda e, q=q, tt=tt, cs=cs: e.tensor_tensor(out=tt_[q][:], in0=bank(tt), in1=grow[:, cs], op=ALU.mult),
                          reads=[("ps", tt), "grow"], writes=[("tt", q)])
                    st.op("dve", lambda e, q=q: e.tensor_tensor(out=tt_[q][:], in0=tt_[q][:], in1=xt[q][:], op=ALU.add),
                          reads=[("tt", q), ("xt", q)], writes=[("tt", q)])
                    st.dma("sp", xres[rows, cs], tt_[q][:], reads=[("tt", q)], writes=[("x", tt, cb)])
            st.flush()

        groups = [(0, LS, [(0, LS)], 0), (LS, LP, [(i * PSEQ, PSEQ) for i in range(NP)], 1)]
        plan = []
        plan.append(("ada", stage_ada, ()))
        for grp in groups:
            for l in range(depth):
                plan.append(("norm1", stage_norm, (l, 1, grp)))
                plan.append(("proj", stage_proj, (l, grp)))
                plan.append(("tr", stage_tr, (grp,)))
                plan.append(("ssd1", stage_ssd1, (l, grp)))
                plan.append(("ssd2", stage_ssd2, (l, grp)))
                plan.append(("o1", stage_o1, (l, grp)))
                plan.append(("o2", stage_o2, (l, grp)))
                plan.append(("wo", stage_wo, (l, grp)))
                plan.append(("norm2", stage_norm, (l, 2, grp)))
                plan.append(("gu", stage_gu, (l, grp)))
                plan.append(("down", stage_down, (l, grp)))
            plan.append(("final", stage_norm, (depth - 1, "f", grp)))
        for nm, fn, args in plan:
            if only is not None and nm not in only:
                continue
            fn(*args)
            if stop_after is not None and nm == stop_after:
                break
    return nc


def make_consts():
    c = np.zeros((128, 4, 128), np.float32)
    i = np.arange(128)
    c[:, 0, :] = np.eye(128, dtype=np.float32)
    c[:, 1, :] = (i[:, None] <= i[None, :]).astype(np.float32)
    c[:, 2, :] = (i[:, None] >= i[None, :]).astype(np.float32)
    c[:, 3, :] = 1.0
    return c


def core_inputs(inputs, core, ROWS=16, NP=4):
    LS = ROWS * GRID_W
    sb = core % 4
    xs = inputs["x_sample"][sb][:LS]
    xp = inputs["x_prompt"][core * NP:(core + 1) * NP].reshape(NP * PSEQ, D)
    m = {
        "xin": np.ascontiguousarray(np.concatenate([xs, xp], axis=0)),
        "cond": np.ascontiguousarray(np.stack([inputs["c"][sb], inputs["c_ctx"]], axis=0).reshape(2, KC, 128).transpose(2, 1, 0)),
        "h0s": np.ascontiguousarray(inputs["state_ssd"][sb].reshape(DEPTH, 2, D, 128)),
        "cst": make_consts(),
    }
    for nm in ["ada_w", "ada_b", "norm1_g", "norm2_g", "w_in", "ssd_d", "ssd_norm_g",
               "ssd_out", "sc_out", "cf_out",
               "w_o", "router_w", "router_bias", "moe_w_gate", "moe_w_up", "moe_w_down", "final_g"]:
        m[nm] = np.ascontiguousarray(inputs[nm])
    for nm in ["ssd_conv_w", "sc_conv_w", "cf_conv_w"]:
        a = inputs[nm]
        m[nm] = np.ascontiguousarray(a.reshape(a.shape[0], a.shape[1], -1, 128).transpose(0, 3, 2, 1))
    for nm in ["ssd_conv_b", "sc_conv_b", "cf_conv_b", "cf_ln_g", "cf_ln_b"]:
        a = inputs[nm]
        m[nm] = np.ascontiguousarray(a.reshape(a.shape[0], -1, 128).transpose(0, 2, 1))
    m["ssd_dt_bias"] = np.ascontiguousarray(inputs["ssd_dt_bias"].reshape(DEPTH, 128))
    m["ssd_a_log"] = np.ascontiguousarray(inputs["ssd_a_log"].reshape(DEPTH, 128))
    return m


def kernel(**inputs):
    inputs = {k: np.asarray(v) for k, v in inputs.items()}
    nc = build()
    in_maps = [core_inputs(inputs, cidx) for cidx in range(8)]
    res = run_bass_kernel_spmd(nc, in_maps, core_ids=list(range(8)))
    LS = 1024
    y_prompt = np.zeros((32, 256, D), np.float32)
    y_sample = np.zeros((4, 1024, D), np.float32)
    nstate = np.zeros((32, DEPTH, 2, 64, 64, 128), np.float32)
    for cidx in range(8):
        r = res.results[cidx]
        if cidx < 4:
            y_sample[cidx] = r["y"][:LS]
        y_prompt[cidx * 4:(cidx + 1) * 4] = r["y"][LS:].reshape(4, 256, D)
        nstate[cidx * 4:(cidx + 1) * 4] = r["nst"].reshape(4, DEPTH, 2, 64, 64, 128)
    return y_prompt, y_sample, nstate
```
